# Optimizing a Trainium2 kernel written in Bass

```python
import math
import jax
import jax.numpy as jnp
from jax import lax
import numpy as np

D_MODEL = 2048
BATCH = 4
SEQ = 2048
DEPTH = 2

N_EVEN = (DEPTH + 1) // 2
N_ODD = DEPTH // 2
MIX_W = D_MODEL
NORM_EPS = 1e-6

RW_HEAD = 64
RW_W = MIX_W // 2
RW_H = RW_W // RW_HEAD
RW_DECAY_LORA = 96
RW_ICLR_LORA = 96
RW_GATE_LORA = 256
RW_IN = 3 * RW_W + RW_DECAY_LORA + RW_ICLR_LORA + RW_GATE_LORA
RW_LN_EPS = 64e-5
RW_DECAY_SCALE = math.exp(-0.5)

ML_W = MIX_W - RW_W
ML_H = 4
ML_DV = ML_W // ML_H
ML_DK = ML_DV // 2
ML_QK = ML_H * ML_DK
ML_CONV = 4
ML_CHUNK = 64
ML_IN = 2 * ML_QK + 2 * ML_W + 2 * ML_H
EV_IN = RW_IN + ML_IN

HG_W = MIX_W // 2
HG_HEAD = 128
HG_H = HG_W // HG_HEAD
HG_CHUNK = 64
HG_IN = 4 * HG_W

MB_W = MIX_W - HG_W
MB_HEAD = 64
MB_H = MB_W // MB_HEAD
MB_G = 4
MB_E = MB_H // MB_G
MB_N = 128
MB_CONV = 4
MB_CHUNK = 64
MB_CONV_W = MB_W + 2 * MB_G * MB_N
MB_IN = MB_W + MB_CONV_W + MB_H
OD_IN = HG_IN + MB_IN

FFN_DENSE = 5632
N_EXPERTS = 8
TOP_K = 2
FFN_EXPERT = 2816

kernel_name = 'hybrid_rwkv7_mlstm_hgrn2_mamba2_moe'


def rmsnorm(x, w, eps=NORM_EPS):
    xf = x.astype(jnp.float32)
    y = xf * lax.rsqrt(jnp.mean(xf * xf, axis=-1, keepdims=True) + eps)
    return (y * w.astype(jnp.float32)).astype(x.dtype)


def head_rmsnorm(x, w):
    H, d = x.shape[-2:]
    return rmsnorm(x, w.reshape(H, d))


def split_cols(u, sizes):
    out, off = [], 0
    for s in sizes:
        out.append(u[..., off:off + s])
        off += s
    return out


def token_shift(x):
    return jnp.pad(x, ((0, 0), (1, 0), (0, 0)))[:, :-1]


def causal_dwconv(x, w, b):
    K, C = w.shape
    y = lax.conv_general_dilated(x, w.astype(x.dtype)[:, None, :], window_strides=(1,),
                                 padding=[(K - 1, 0)], dimension_numbers=('NWC', 'WIO', 'NWC'),
                                 feature_group_count=C)
    return y + b.astype(x.dtype)


def segsum(a):
    L = a.shape[-1]
    xr = jnp.broadcast_to(a[..., :, None], a.shape + (L,))
    xr = jnp.where(jnp.tril(jnp.ones((L, L), bool), -1), xr, 0.0)
    xr = jnp.cumsum(xr, axis=-2)
    return jnp.where(jnp.tril(jnp.ones((L, L), bool)), xr, -jnp.inf)


def rwkv7_group(u, mu, w0, w2, a0, a2, g2, k_k, k_a, r_k, ln_w, ln_b):
    Bsz, T, _ = u.shape
    uf = u.astype(jnp.float32)
    uf = uf + (token_shift(uf) - uf) * mu
    r, k, v, dw, da, dg = split_cols(uf, [RW_W, RW_W, RW_W, RW_DECAY_LORA, RW_ICLR_LORA, RW_GATE_LORA])
    log_w = -RW_DECAY_SCALE * jax.nn.sigmoid(w0 + jnp.tanh(dw) @ w2)
    a = jax.nn.sigmoid(a0 + da @ a2)
    g = jax.nn.sigmoid(dg) @ g2
    hs = lambda t: t.reshape(Bsz, T, RW_H, RW_HEAD)
    kk = hs(k * k_k)
    kk = kk * lax.rsqrt(jnp.maximum(jnp.sum(kk * kk, -1, keepdims=True), 1e-12))
    k = k * (1.0 + (a - 1.0) * k_a)
    r_h, k_h, v_h, a_h, w_h = hs(r), hs(k), hs(v), hs(a), jnp.exp(hs(log_w))

    def step(S, inp):
        r_t, w_t, k_t, v_t, kk_t, a_t = inp
        sa = jnp.einsum('bhij,bhj->bhi', S, -kk_t)
        S = (S * w_t[:, :, None, :] + sa[..., None] * (kk_t * a_t)[:, :, None, :]
             + v_t[..., None] * k_t[:, :, None, :])
        return S, jnp.einsum('bhij,bhj->bhi', S, r_t)

    tm = lambda t: jnp.moveaxis(t, 1, 0)
    S0 = jnp.zeros((Bsz, RW_H, RW_HEAD, RW_HEAD), jnp.float32)
    _, y = lax.scan(step, S0, (tm(r_h), tm(w_h), tm(k_h), tm(v_h), tm(kk), tm(a_h)))
    y = jnp.moveaxis(y, 0, 1)
    mean = jnp.mean(y, -1, keepdims=True)
    var = jnp.mean(jnp.square(y - mean), -1, keepdims=True)
    y = (y - mean) * lax.rsqrt(var + RW_LN_EPS) * ln_w.reshape(RW_H, RW_HEAD) + ln_b.reshape(RW_H, RW_HEAD)
    y = y + jnp.sum(r_h * k_h * r_k, -1, keepdims=True) * v_h
    return (y.reshape(Bsz, T, RW_W) * g).astype(u.dtype)


def mlstm_chunkwise(q, k, v, i_log, f_log):
    Bsz, H, T, DK = q.shape
    DV = v.shape[-1]
    L = ML_CHUNK
    NC = T // L
    q = q.reshape(Bsz, H, NC, L, DK)
    k = k.reshape(Bsz, H, NC, L, DK)
    v = v.reshape(Bsz, H, NC, L, DV)
    i_log = i_log.reshape(Bsz, H, NC, L)
    b = jnp.cumsum(f_log.reshape(Bsz, H, NC, L), -1)
    g = b[..., -1]
    w_state = g[..., None] - b + i_log
    a_loc = jnp.max(w_state, -1)
    e = jnp.exp(w_state - a_loc[..., None])
    kv_loc = jnp.einsum('bhcl,bhclk,bhclv->bhckv', e, k, v)
    n_loc = jnp.einsum('bhcl,bhclk->bhck', e, k)

    def step(carry, inp):
        C, n, m = carry
        g_c, a_c, kv_c, n_c = inp
        m_new = jnp.maximum(g_c + m, a_c)
        s_old = jnp.exp(g_c + m - m_new)
        s_loc = jnp.exp(a_c - m_new)
        C_new = s_old[..., None, None] * C + s_loc[..., None, None] * kv_c
        n_new = s_old[..., None] * n + s_loc[..., None] * n_c
        return (C_new, n_new, m_new), (C, n, m)

    init = (jnp.zeros((Bsz, H, DK, DV), jnp.float32), jnp.zeros((Bsz, H, DK), jnp.float32),
            jnp.zeros((Bsz, H), jnp.float32))
    cm = lambda t: jnp.moveaxis(t, 2, 0)
    _, (C_prev, n_prev, m_prev) = lax.scan(step, init, (cm(g), cm(a_loc), cm(kv_loc), cm(n_loc)))
    C_prev = jnp.moveaxis(C_prev, 0, 2)
    n_prev = jnp.moveaxis(n_prev, 0, 2)
    m_prev = jnp.moveaxis(m_prev, 0, 2)
    causal = jnp.tril(jnp.ones((L, L), bool))
    D = jnp.where(causal, b[..., :, None] - b[..., None, :] + i_log[..., None, :], -jnp.inf)
    inter_log = b + m_prev[..., None]
    m_t = jnp.maximum(inter_log, jnp.max(D, -1))
    inter_s = jnp.exp(inter_log - m_t)
    qk = jnp.einsum('bhctk,bhcsk->bhcts', q, k) * jnp.exp(D - m_t[..., None])
    num = inter_s[..., None] * jnp.einsum('bhctk,bhckv->bhctv', q, C_prev) + jnp.einsum('bhcts,bhcsv->bhctv', qk, v)
    den = inter_s * jnp.einsum('bhctk,bhck->bhct', q, n_prev) + jnp.sum(qk, -1)
    h = num / jnp.maximum(jnp.abs(den), jnp.exp(-m_t))[..., None]
    return h.reshape(Bsz, H, T, DV)


def mlstm_group(u, conv_w, conv_b, i_b, f_b, norm_w):
    Bsz, T, _ = u.shape
    qk, v, o, i_pre, f_pre = split_cols(u, [2 * ML_QK, ML_W, ML_W, ML_H, ML_H])
    qk = jax.nn.silu(causal_dwconv(qk, conv_w, conv_b)).astype(jnp.float32)
    heads = lambda t, d: jnp.moveaxis(t.reshape(Bsz, T, ML_H, d), 2, 1)
    q = heads(qk[..., :ML_QK], ML_DK) * (ML_DK ** -0.5)
    k = heads(qk[..., ML_QK:], ML_DK)
    v = heads(v.astype(jnp.float32), ML_DV)
    i_log = jnp.moveaxis(i_pre.astype(jnp.float32) + i_b, 2, 1)
    f_log = jax.nn.log_sigmoid(jnp.moveaxis(f_pre.astype(jnp.float32) + f_b, 2, 1))
    h = mlstm_chunkwise(q, k, v, i_log, f_log)
    h = head_rmsnorm(jnp.moveaxis(h, 1, 2), norm_w).reshape(Bsz, T, ML_W)
    return (h * jax.nn.sigmoid(o.astype(jnp.float32))).astype(u.dtype)


def hgrn2_chunkwise(q, k, i, log_f):
    Bsz, H, T, DK = q.shape
    DV = i.shape[-1]
    L = HG_CHUNK
    NC = T // L
    to_chunks = lambda t: jnp.moveaxis(t.reshape(Bsz, H, NC, L, t.shape[-1]), 2, 0)
    causal = jnp.tril(jnp.ones((L, L), bool))[:, :, None]

    def step(S, inp):
        q_c, k_c, i_c, lf_c = inp
        bcum = jnp.cumsum(lf_c, axis=2)
        o_inter = jnp.einsum('bhtk,bhkv->bhtv', q_c * jnp.exp(bcum), S)
        diff = bcum[:, :, :, None, :] - bcum[:, :, None, :, :]
        decay = jnp.exp(jnp.where(causal, diff, -jnp.inf))
        attn = jnp.einsum('bhtk,bhsk,bhtsk->bhts', q_c, k_c, decay)
        o_intra = jnp.einsum('bhts,bhsv->bhtv', attn, i_c)
        b_last = bcum[:, :, -1, :]
        S_new = (jnp.exp(b_last)[..., None] * S
                 + jnp.einsum('bhsk,bhsv->bhkv', k_c * jnp.exp(b_last[:, :, None, :] - bcum), i_c))
        return S_new, o_inter + o_intra

    S0 = jnp.zeros((Bsz, H, DK, DV), jnp.float32)
    _, o = lax.scan(step, S0, (to_chunks(q), to_chunks(k), to_chunks(i), to_chunks(log_f)))
    return jnp.moveaxis(o, 0, 2).reshape(Bsz, H, T, DV)


def hgrn2_group(u, lb, norm_w):
    Bsz, T, _ = u.shape
    q, f_pre, i, g = split_cols(u.astype(jnp.float32), [HG_W, HG_W, HG_W, HG_W])
    q = jax.nn.silu(q)
    lb = lb.astype(jnp.float32)
    log_f = jnp.logaddexp(jnp.log(lb), jnp.log1p(-lb) + jax.nn.log_sigmoid(f_pre))
    k = (1.0 - lb) * jax.nn.sigmoid(-f_pre)
    heads = lambda t: jnp.moveaxis(t.reshape(Bsz, T, HG_H, HG_HEAD), 2, 1)
    o = hgrn2_chunkwise(heads(q), heads(k), heads(i), heads(log_f))
    o = head_rmsnorm(jnp.moveaxis(o, 1, 2), norm_w).reshape(Bsz, T, HG_W)
    return (o * jax.nn.silu(g)).astype(u.dtype)


def ssd_chunked(X, Adt, Bm, Cm):
    Bsz, T, G, E, P = X.shape
    N = Bm.shape[-1]
    L = MB_CHUNK
    NC = T // L
    X = X.reshape(Bsz, NC, L, G, E, P)
    Bm = Bm.reshape(Bsz, NC, L, G, N)
    Cm = Cm.reshape(Bsz, NC, L, G, N)
    Adt = jnp.moveaxis(Adt.reshape(Bsz, NC, L, G, E), (1, 2), (3, 4))
    A_cum = jnp.cumsum(Adt, -1)
    Lmat = jnp.exp(segsum(Adt))
    CB = jnp.einsum('bclgn,bcsgn->bgcls', Cm, Bm)
    Y_diag = jnp.einsum('bgecls,bcsgep->bclgep', CB[:, :, None] * Lmat, X)
    decay_states = jnp.exp(A_cum[..., -1:] - A_cum)
    states = jnp.einsum('bclgn,bgecl,bclgep->bcgepn', Bm, decay_states, X)
    states = jnp.concatenate([jnp.zeros_like(states[:, :1]), states], axis=1)
    decay_chunk = jnp.exp(segsum(jnp.pad(A_cum[..., -1], ((0, 0), (0, 0), (0, 0), (1, 0)))))
    states = jnp.einsum('bgezc,bcgepn->bzgepn', decay_chunk, states)[:, :-1]
    Y_off = jnp.einsum('bclgn,bcgepn,bgecl->bclgep', Cm, states, jnp.exp(A_cum))
    return (Y_diag + Y_off).reshape(Bsz, T, G, E, P)


def mamba2_group(u, conv_w, conv_b, dt_bias, A_log, D_skip, norm_w):
    Bsz, T, _ = u.shape
    z, xBC, dt = split_cols(u, [MB_W, MB_CONV_W, MB_H])
    xBC = jax.nn.silu(causal_dwconv(xBC, conv_w, conv_b)).astype(jnp.float32)
    xs, Bm, Cm = split_cols(xBC, [MB_W, MB_G * MB_N, MB_G * MB_N])
    dt = jax.nn.softplus(dt.astype(jnp.float32) + dt_bias)
    A = -jnp.exp(A_log.astype(jnp.float32))
    xh = xs.reshape(Bsz, T, MB_G, MB_E, MB_HEAD)
    dth = dt.reshape(Bsz, T, MB_G, MB_E)
    y = ssd_chunked(xh * dth[..., None], (dt * A).reshape(Bsz, T, MB_G, MB_E),
                    Bm.reshape(Bsz, T, MB_G, MB_N), Cm.reshape(Bsz, T, MB_G, MB_N))
    y = y + xh * D_skip.reshape(MB_G, MB_E)[..., None]
    y = y.reshape(Bsz, T, MB_W) * jax.nn.silu(z.astype(jnp.float32))
    y = rmsnorm(y.reshape(Bsz, T, MB_G, MB_W // MB_G), norm_w.reshape(MB_G, MB_W // MB_G))
    return y.reshape(Bsz, T, MB_W).astype(u.dtype)


def swiglu(h, w_gate, w_up, w_down):
    return (jax.nn.silu(h @ w_gate) * (h @ w_up)) @ w_down


def moe_swiglu(h, router, w_gate, w_up, w_down):
    logits = (h @ router).astype(jnp.float32)
    top_val, top_idx = lax.top_k(logits, TOP_K)
    top_p = jax.nn.softmax(top_val, axis=-1)
    gates = jnp.einsum('btk,btke->bte', top_p, jax.nn.one_hot(top_idx, N_EXPERTS, dtype=jnp.float32))
    out = jnp.zeros_like(h)
    for e in range(N_EXPERTS):
        out = out + gates[..., e:e + 1].astype(h.dtype) * swiglu(h, w_gate[e], w_up[e], w_down[e])
    return out


def setup_inputs(seed: int = 0) -> dict:
    key = jax.random.key(seed)
    ks = iter(jax.random.split(key, 64))
    nrm = lambda shape, scale: scale * jax.random.normal(next(ks), shape, jnp.float32)
    unif = lambda shape, lo, hi: jax.random.uniform(next(ks), shape, jnp.float32, lo, hi)
    gain = lambda shape: 1.0 + nrm(shape, 0.02)
    E, O = N_EVEN, N_ODD
    x = nrm((BATCH, SEQ, D_MODEL), 1.0)
    final_norm_w = gain((D_MODEL,))
    hg_lb_logits = nrm((DEPTH, HG_W), 0.3)
    ev_norm1_w = gain((E, D_MODEL))
    ev_w_in = nrm((E, D_MODEL, EV_IN), D_MODEL ** -0.5)
    ev_w_out = nrm((E, MIX_W, D_MODEL), MIX_W ** -0.5)
    rw_mu = unif((E, RW_IN), 0.0, 1.0)
    rw_w0 = jnp.linspace(-6.0, -0.5, RW_W)[None] + nrm((E, RW_W), 0.1)
    rw_w2 = nrm((E, RW_DECAY_LORA, RW_W), 0.5 * RW_DECAY_LORA ** -0.5)
    rw_a0 = nrm((E, RW_W), 0.1)
    rw_a2 = nrm((E, RW_ICLR_LORA, RW_W), RW_ICLR_LORA ** -0.5)
    rw_g2 = nrm((E, RW_GATE_LORA, RW_W), RW_GATE_LORA ** -0.5)
    rw_k_k = 0.85 + nrm((E, RW_W), 0.02)
    rw_k_a = 1.0 + nrm((E, RW_W), 0.02)
    rw_r_k = nrm((E, RW_H, RW_HEAD), 0.1)
    rw_ln_w = gain((E, RW_W))
    rw_ln_b = nrm((E, RW_W), 0.02)
    ml_conv_w = nrm((E, ML_CONV, 2 * ML_QK), 0.5)
    ml_conv_b = nrm((E, 2 * ML_QK), 0.02)
    ml_i_b = nrm((E, ML_H), 0.1)
    ml_f_b = jnp.linspace(3.0, 6.0, ML_H)[None] + nrm((E, ML_H), 0.1)
    ml_norm_w = gain((E, ML_W))
    ev_norm2_w = gain((E, D_MODEL))
    ffn_w_gate = nrm((E, D_MODEL, FFN_DENSE), D_MODEL ** -0.5)
    ffn_w_up = nrm((E, D_MODEL, FFN_DENSE), D_MODEL ** -0.5)
    ffn_w_down = nrm((E, FFN_DENSE, D_MODEL), FFN_DENSE ** -0.5)
    od_norm1_w = gain((O, D_MODEL))
    od_w_in = nrm((O, D_MODEL, OD_IN), D_MODEL ** -0.5)
    od_w_out = nrm((O, MIX_W, D_MODEL), MIX_W ** -0.5)
    hg_norm_w = gain((O, HG_W))
    mb_conv_w = nrm((O, MB_CONV, MB_CONV_W), 0.5)
    mb_conv_b = nrm((O, MB_CONV_W), 0.02)
    dt0 = jnp.exp(unif((O, MB_H), math.log(1e-3), math.log(1e-1)))
    mb_dt_bias = dt0 + jnp.log(-jnp.expm1(-dt0))
    mb_A_log = jnp.log(unif((O, MB_H), 1.0, 16.0))
    mb_D = gain((O, MB_H))
    mb_norm_w = gain((O, MB_W))
    od_norm2_w = gain((O, D_MODEL))
    moe_router = nrm((O, D_MODEL, N_EXPERTS), D_MODEL ** -0.5)
    moe_w_gate = nrm((O, N_EXPERTS, D_MODEL, FFN_EXPERT), D_MODEL ** -0.5)
    moe_w_up = nrm((O, N_EXPERTS, D_MODEL, FFN_EXPERT), D_MODEL ** -0.5)
    moe_w_down = nrm((O, N_EXPERTS, FFN_EXPERT, D_MODEL), FFN_EXPERT ** -0.5)
    return {'x': x, 'final_norm_w': final_norm_w, 'hg_lb_logits': hg_lb_logits,
            'ev_norm1_w': ev_norm1_w, 'ev_w_in': ev_w_in, 'ev_w_out': ev_w_out,
            'rw_mu': rw_mu, 'rw_w0': rw_w0, 'rw_w2': rw_w2, 'rw_a0': rw_a0, 'rw_a2': rw_a2,
            'rw_g2': rw_g2, 'rw_k_k': rw_k_k, 'rw_k_a': rw_k_a, 'rw_r_k': rw_r_k,
            'rw_ln_w': rw_ln_w, 'rw_ln_b': rw_ln_b,
            'ml_conv_w': ml_conv_w, 'ml_conv_b': ml_conv_b, 'ml_i_b': ml_i_b, 'ml_f_b': ml_f_b,
            'ml_norm_w': ml_norm_w,
            'ev_norm2_w': ev_norm2_w, 'ffn_w_gate': ffn_w_gate, 'ffn_w_up': ffn_w_up, 'ffn_w_down': ffn_w_down,
            'od_norm1_w': od_norm1_w, 'od_w_in': od_w_in, 'od_w_out': od_w_out,
            'hg_norm_w': hg_norm_w,
            'mb_conv_w': mb_conv_w, 'mb_conv_b': mb_conv_b, 'mb_dt_bias': mb_dt_bias,
            'mb_A_log': mb_A_log, 'mb_D': mb_D, 'mb_norm_w': mb_norm_w,
            'od_norm2_w': od_norm2_w, 'moe_router': moe_router, 'moe_w_gate': moe_w_gate,
            'moe_w_up': moe_w_up, 'moe_w_down': moe_w_down}


def reference(x, final_norm_w, hg_lb_logits,
              ev_norm1_w, ev_w_in, ev_w_out,
              rw_mu, rw_w0, rw_w2, rw_a0, rw_a2, rw_g2, rw_k_k, rw_k_a, rw_r_k, rw_ln_w, rw_ln_b,
              ml_conv_w, ml_conv_b, ml_i_b, ml_f_b, ml_norm_w,
              ev_norm2_w, ffn_w_gate, ffn_w_up, ffn_w_down,
              od_norm1_w, od_w_in, od_w_out,
              hg_norm_w,
              mb_conv_w, mb_conv_b, mb_dt_bias, mb_A_log, mb_D, mb_norm_w,
              od_norm2_w, moe_router, moe_w_gate, moe_w_up, moe_w_down):
    p = jax.nn.softmax(hg_lb_logits.astype(jnp.float32), axis=0)
    lower_bounds = jnp.cumsum(p, axis=0) - p[0]
    for layer in range(DEPTH):
        j = layer // 2
        if layer % 2 == 0:
            u = rmsnorm(x, ev_norm1_w[j]) @ ev_w_in[j]
            y_a = rwkv7_group(u[..., :RW_IN], rw_mu[j], rw_w0[j], rw_w2[j], rw_a0[j], rw_a2[j], rw_g2[j],
                              rw_k_k[j], rw_k_a[j], rw_r_k[j], rw_ln_w[j], rw_ln_b[j])
            y_b = mlstm_group(u[..., RW_IN:], ml_conv_w[j], ml_conv_b[j], ml_i_b[j], ml_f_b[j], ml_norm_w[j])
            x = x + jnp.concatenate([y_a, y_b], axis=-1) @ ev_w_out[j]
            x = x + swiglu(rmsnorm(x, ev_norm2_w[j]), ffn_w_gate[j], ffn_w_up[j], ffn_w_down[j])
        else:
            u = rmsnorm(x, od_norm1_w[j]) @ od_w_in[j]
            y_c = hgrn2_group(u[..., :HG_IN], lower_bounds[layer], hg_norm_w[j])
            y_d = mamba2_group(u[..., HG_IN:], mb_conv_w[j], mb_conv_b[j], mb_dt_bias[j], mb_A_log[j],
                               mb_D[j], mb_norm_w[j])
            x = x + jnp.concatenate([y_c, y_d], axis=-1) @ od_w_out[j]
            x = x + moe_swiglu(rmsnorm(x, od_norm2_w[j]), moe_router[j], moe_w_gate[j], moe_w_up[j], moe_w_down[j])
    return rmsnorm(x, final_norm_w)
```

```python
import math
from contextlib import ExitStack

import numpy as np
import concourse.bass as bass
import concourse.mybir as mybir
from concourse.bass_utils import run_bass_kernel_spmd

F32 = mybir.dt.float32
BF16 = mybir.dt.bfloat16
AF = mybir.ActivationFunctionType
ALU = mybir.AluOpType
AX = mybir.AxisListType

D = 2048
T = 2048
NB = 4
KC = D // 128
EPS = 1e-6


class Tile:
    def __init__(self, ap, name="", psum=False):
        self.ap = ap
        self.name = name
        self.psum = psum
        self.last_w = None
        self.readers = []
        self.dsem = None
        self.dcount = 0

    def __getitem__(self, idx):
        return self.ap[idx]


class Prog:
    ENGS = ("tensor", "vector", "scalar", "gpsimd", "sync")

    def __init__(self, nc, stack):
        self.nc = nc
        self.stack = stack
        self.ops = {e: [] for e in self.ENGS}
        self.esem = {}
        self.ndsem = 0
        self.nt = 0

    def sb(self, shape, dtype, name=None):
        self.nt += 1
        name = name or f"sb{self.nt}"
        h = self.stack.enter_context(self.nc.sbuf_tensor(name, list(shape), dtype))
        return h

    def ps(self, shape, dtype=F32, name=None):
        self.nt += 1
        name = name or f"ps{self.nt}"
        h = self.stack.enter_context(self.nc.psum_tensor(name, list(shape), dtype))
        return h

    def tile(self, ap, name=""):
        return Tile(ap, name)

    def sbt(self, shape, dtype, name=None):
        h = self.sb(shape, dtype, name)
        return Tile(h[tuple(slice(None) for _ in shape)], name or "")

    def pst(self, shape, dtype=F32, name=None):
        h = self.ps(shape, dtype, name)
        return Tile(h[tuple(slice(None) for _ in shape)], name or "", psum=True)

    def _deps(self, eng, reads, writes):
        waits = []
        for t in reads:
            if t.last_w is not None:
                waits.append(t.last_w)
            if t.psum:
                waits.extend(r for r in t.readers if not (r[0] == "e" and r[1] == eng))
        for t in writes:
            if t.last_w is not None:
                waits.append(t.last_w)
            waits.extend(t.readers)
        out = []
        for w in waits:
            if w[0] == "e" and w[1] == eng and eng == "tensor":
                continue
            out.append(w)
        return out

    def _commit(self, tok, reads, writes):
        for t in writes:
            t.last_w = tok
            t.readers = []
        for t in reads:
            if t not in writes:
                t.readers.append(tok)

    def op(self, eng, fn, reads=(), writes=()):
        reads = list(reads)
        writes = list(writes)
        waits = self._deps(eng, reads, writes)
        seq = len(self.ops[eng])
        tok = ("e", eng, seq)
        self.ops[eng].append(dict(fn=fn, waits=waits, sig=False, dma=None))
        self._commit(tok, reads, writes)
        return tok

    def dma(self, eng, out_ap, in_ap, owner, reads=(), writes=(), **kw):
        reads = list(reads)
        writes = list(writes)
        waits = self._deps(eng, reads, writes)
        if owner.dsem is None:
            owner.dsem = self.stack.enter_context(self.nc.semaphore(f"dsem{self.ndsem}"))
            self.ndsem += 1
        owner.dcount += 16
        tok = ("d", owner, owner.dcount)

        def fn(e, out_ap=out_ap, in_ap=in_ap, kw=kw):
            return e.dma_start(out=out_ap, in_=in_ap, **kw)

        self.ops[eng].append(dict(fn=fn, waits=waits, sig=False, dma=owner))
        self._commit(tok, reads, writes)
        return tok

    def emit(self, final_waits=()):
        nc = self.nc
        for e in self.ENGS:
            self.esem[e] = self.stack.enter_context(nc.semaphore(f"esem_{e}"))
        for e in self.ENGS:
            for o in self.ops[e]:
                for w in o["waits"]:
                    if w[0] == "e":
                        self.ops[w[1]][w[2]]["sig"] = True
        for w in final_waits:
            if w[0] == "e":
                self.ops[w[1]][w[2]]["sig"] = True
        sigval = {}
        for e in self.ENGS:
            c = 0
            for i, o in enumerate(self.ops[e]):
                if o["sig"]:
                    c += 1
                    sigval[(e, i)] = c

        def resolve(w):
            if w[0] == "e":
                return self.esem[w[1]], sigval[(w[1], w[2])]
            return w[1].dsem, w[2]

        block = self.stack.enter_context(nc.Block())

        def make(e):
            def body(engobj):
                waited = {}
                for o in self.ops[e]:
                    for w in o["waits"]:
                        sem, val = resolve(w)
                        k = id(sem)
                        if waited.get(k, 0) < val:
                            engobj.wait_ge(sem, val)
                            waited[k] = val
                    ins = o["fn"](engobj)
                    if o["dma"] is not None:
                        ins.then_inc(o["dma"].dsem, 16)
                    elif o["sig"]:
                        ins.then_inc(self.esem[e], 1)
                if e == "sync":
                    for w in final_waits:
                        sem, val = resolve(w)
                        engobj.wait_ge(sem, val)
            return body

        block.tensor(make("tensor"))
        block.vector(make("vector"))
        block.scalar(make("scalar"))
        block.gpsimd(make("gpsimd"))
        block.sync(make("sync"))


def rsqrt_op(P, dst_t, dst_ap, src_t, src_ap, scale, bias_t):
    P.op("scalar", lambda e: e.activation(dst_ap, src_ap, AF.Sqrt, bias=bias_t[0:dst_ap.shape[0], 0:1], scale=scale),
         reads=[src_t, bias_t], writes=[dst_t])
    P.op("vector", lambda e: e.reciprocal(dst_ap, dst_ap), reads=[dst_t], writes=[dst_t])


class Rot:
    def __init__(self, tiles):
        self.tiles = tiles
        self.i = 0

    def next(self):
        t = self.tiles[self.i % len(self.tiles)]
        self.i += 1
        return t


def build_phase_b(NT, n_exp, F, moe, final_norm):
    nc = bass.Bass("TRN2", target_bir_lowering=False)
    FB = F // 128
    NTB = NT // 512
    dr = lambda name, shape, dt=F32: nc.dram_tensor(name, list(shape), dt, kind="ExternalInput").ap()
    xT = dr("xT", [D, NT])
    yT = dr("yT", [D, NT])
    w_out = dr("w_out", [D, D])
    norm2 = dr("norm2", [128, KC])
    wg = dr("wg", [n_exp, D, F])
    wu = dr("wu", [n_exp, D, F])
    wd = dr("wd", [n_exp, F, D])
    if moe:
        router = dr("router", [D, 8])
    if final_norm:
        fnorm = dr("fnorm", [128, KC])
    outT = nc.dram_tensor("outT", [D, NT], F32, kind="ExternalOutput").ap()

    with ExitStack() as stack:
        P = Prog(nc, stack)
        X_h = P.sb([128, KC, NT], F32, "X")
        YX_h = P.sb([128, KC, NT], BF16, "YX")
        Xt = [[Tile(X_h[:, m, n * 512:(n + 1) * 512]) for n in range(NTB)] for m in range(KC)]
        YXt = [Tile(YX_h[:, :, n * 512:(n + 1) * 512]) for n in range(NTB)]
        nw_t = P.sbt([128, KC], F32, "nw")
        ones_t = P.sbt([128, 128], BF16, "ones")
        eps_t = P.sbt([128, 1], F32, "eps")
        P.op("vector", lambda e: e.memset(eps_t[:, :], EPS), writes=[eps_t])
        QF = 11
        A_tiles = [[Tile(P.sb([128, QF, 512], BF16, f"A{i}_{n}")[:, :, :]) for n in range(NTB)] for i in range(2)]
        wg_rot = Rot([P.sbt([128, KC, 128], BF16, f"wg{i}") for i in range(3)])
        wo_rot = wg_rot
        wu_rot = Rot([P.sbt([128, KC, 128], BF16, f"wu{i}") for i in range(3)])
        wd_rot = Rot([P.sbt([128, QF, 128], BF16, f"wd{i}") for i in range(3)])
        ps_rot = Rot([P.pst([128, 512], F32, f"psb{i}") for i in range(8)])
        sq_rot = Rot([P.sbt([128, 512], BF16, f"sq{i}") for i in range(2)])
        sg_rot = Rot([P.sbt([128, 512], F32, f"sg{i}") for i in range(3)])
        rstd_t = [P.sbt([128, 512], F32, f"rstd{n}") for n in range(NTB)]
        if moe:
            rt_t = P.sbt([128, KC, 8], BF16, "router_sb")
            ident_t = P.sbt([128, 128], BF16, "ident")
            gate_bc = [[P.sbt([128, 512], BF16, f"gbc{e}_{n}") for n in range(NTB)] for e in range(8)]

        xT_v = xT.rearrange("(kc p) t -> p kc t", p=128)
        yT_v = yT.rearrange("(kc p) t -> p kc t", p=128)
        for m in range(KC):
            for n in range(NTB):
                P.dma("sync", Xt[m][n][:, :], xT_v[:, m, n * 512:(n + 1) * 512], owner=Xt[m][n], writes=[Xt[m][n]])
        for n in range(NTB):
            for k0 in range(0, KC, 4):
                P.dma("gpsimd", YXt[n][:, k0:k0 + 4, :], yT_v[:, k0:k0 + 4, n * 512:(n + 1) * 512], owner=YXt[n], writes=[YXt[n]])
        P.dma("sync", nw_t[:, :], norm2, owner=nw_t, writes=[nw_t])
        P.op("vector", lambda e: e.memset(ones_t[:, :], 1.0), writes=[ones_t])

        wo_v = w_out.rearrange("(kc p) d -> p kc d", p=128)
        for m in range(KC):
            wt = wo_rot.next()
            P.dma("gpsimd", wt[:, :, :], wo_v[:, :, m * 128:(m + 1) * 128], owner=wt, writes=[wt])
            for n in range(NTB):
                pt = ps_rot.next()
                for kc in range(KC):
                    P.op("tensor", lambda e, pt=pt, wt=wt, kc=kc, n=n: e.matmul(
                        pt[:, :], wt[:, kc, :], YXt[n][:, kc, :], start=(kc == 0), stop=(kc == KC - 1)),
                        reads=[wt, YXt[n]], writes=[pt])
                xt = Xt[m][n]
                P.op("vector", lambda e, xt=xt, pt=pt: e.tensor_tensor(xt[:, :], xt[:, :], pt[:, :], ALU.add),
                     reads=[pt, xt], writes=[xt])

        def rmsnorm_into(dst_tiles, w_tile):
            for n in range(NTB):
                pt = ps_rot.next()
                for kc in range(KC):
                    sq = sq_rot.next()
                    P.op("scalar", lambda e, sq=sq, kc=kc, n=n: e.activation(sq[:, :], Xt[kc][n][:, :], AF.Square),
                         reads=[Xt[kc][n]], writes=[sq])
                    P.op("tensor", lambda e, pt=pt, sq=sq, kc=kc: e.matmul(
                        pt[:, :], ones_t[:, :], sq[:, :], start=(kc == 0), stop=(kc == KC - 1)),
                        reads=[ones_t, sq], writes=[pt])
                rs = rstd_t[n]
                rsqrt_op(P, rs, rs[:, :], pt, pt[:, :], 1.0 / D, eps_t)
                for kc in range(KC):
                    P.op("vector", lambda e, kc=kc, n=n, rs=rs: e.scalar_tensor_tensor(
                        dst_tiles[n][:, kc, :] if dst_tiles[n].ap.ndim == 3 else dst_tiles[n][:, :],
                        Xt[kc][n][:, :], w_tile[:, kc:kc + 1], rs[:, :], ALU.mult, ALU.mult),
                        reads=[Xt[kc][n], w_tile, rs], writes=[dst_tiles[n]])

        rmsnorm_into(YXt, nw_t)

        if moe:
            build_router(P, nc, router, rt_t, ident_t, YXt, gate_bc, ps_rot, NTB)

        for ex in range(n_exp):
            wg_v = wg[ex].rearrange("(kc p) f -> p kc f", p=128)
            wu_v = wu[ex].rearrange("(kc p) f -> p kc f", p=128)
            wd_v = wd[ex].rearrange("(j p) d -> p j d", p=128)
            for q in range(FB // QF):
                At = A_tiles[(ex * (FB // QF) + q) % 2]
                for jj in range(QF):
                    j = q * QF + jj
                    wgt = wg_rot.next()
                    wut = wu_rot.next()
                    P.dma("gpsimd", wgt[:, :, :], wg_v[:, :, j * 128:(j + 1) * 128], owner=wgt, writes=[wgt])
                    P.dma("gpsimd", wut[:, :, :], wu_v[:, :, j * 128:(j + 1) * 128], owner=wut, writes=[wut])
                    for n in range(NTB):
                        pg = ps_rot.next()
                        pu = ps_rot.next()
                        for kc in range(KC):
                            P.op("tensor", lambda e, pg=pg, wgt=wgt, kc=kc, n=n: e.matmul(
                                pg[:, :], wgt[:, kc, :], YXt[n][:, kc, :], start=(kc == 0), stop=(kc == KC - 1)),
                                reads=[wgt, YXt[n]], writes=[pg])
                        for kc in range(KC):
                            P.op("tensor", lambda e, pu=pu, wut=wut, kc=kc, n=n: e.matmul(
                                pu[:, :], wut[:, kc, :], YXt[n][:, kc, :], start=(kc == 0), stop=(kc == KC - 1)),
                                reads=[wut, YXt[n]], writes=[pu])
                        sg = sg_rot.next()
                        P.op("scalar", lambda e, sg=sg, pg=pg: e.activation(sg[:, :], pg[:, :], AF.Silu),
                             reads=[pg], writes=[sg])
                        if moe:
                            P.op("vector", lambda e, sg=sg, ex=ex, n=n: e.tensor_tensor(
                                sg[:, :], sg[:, :], gate_bc[ex][n][:, :], ALU.mult),
                                reads=[sg, gate_bc[ex][n]], writes=[sg])
                        P.op("vector", lambda e, At=At, n=n, jj=jj, sg=sg, pu=pu: e.tensor_tensor(
                            At[n][:, jj, :], sg[:, :], pu[:, :], ALU.mult),
                            reads=[sg, pu], writes=[At[n]])
                for m in range(KC):
                    wdt = wd_rot.next()
                    P.dma("gpsimd", wdt[:, :, :], wd_v[:, q * QF:(q + 1) * QF, m * 128:(m + 1) * 128], owner=wdt, writes=[wdt])
                    for n in range(NTB):
                        pt = ps_rot.next()
                        for jj in range(QF):
                            P.op("tensor", lambda e, pt=pt, wdt=wdt, jj=jj, n=n, At=At: e.matmul(
                                pt[:, :], wdt[:, jj, :], At[n][:, jj, :], start=(jj == 0), stop=(jj == QF - 1)),
                                reads=[wdt, At[n]], writes=[pt])
                        xt = Xt[m][n]
                        P.op("vector", lambda e, xt=xt, pt=pt: e.tensor_tensor(xt[:, :], xt[:, :], pt[:, :], ALU.add),
                             reads=[pt, xt], writes=[xt])

        out_v = outT.rearrange("(kc p) t -> p kc t", p=128)
        finals = []
        if final_norm:
            fw_t = P.sbt([128, KC], F32, "fw")
            P.dma("sync", fw_t[:, :], fnorm, owner=fw_t, writes=[fw_t])
            for n in range(NTB):
                pt = ps_rot.next()
                for kc in range(KC):
                    sq = sq_rot.next()
                    P.op("scalar", lambda e, sq=sq, kc=kc, n=n: e.activation(sq[:, :], Xt[kc][n][:, :], AF.Square),
                         reads=[Xt[kc][n]], writes=[sq])
                    P.op("tensor", lambda e, pt=pt, sq=sq, kc=kc: e.matmul(
                        pt[:, :], ones_t[:, :], sq[:, :], start=(kc == 0), stop=(kc == KC - 1)),
                        reads=[ones_t, sq], writes=[pt])
                rs = rstd_t[n]
                rsqrt_op(P, rs, rs[:, :], pt, pt[:, :], 1.0 / D, eps_t)
                for kc in range(KC):
                    xt = Xt[kc][n]
                    P.op("vector", lambda e, xt=xt, kc=kc, rs=rs: e.scalar_tensor_tensor(
                        xt[:, :], xt[:, :], fw_t[:, kc:kc + 1], rs[:, :], ALU.mult, ALU.mult),
                        reads=[xt, fw_t, rs], writes=[xt])
        for m in range(KC):
            for n in range(NTB):
                xt = Xt[m][n]
                finals.append(P.dma("sync", out_v[:, m, n * 512:(n + 1) * 512], xt[:, :], owner=xt, reads=[xt]))
        P.emit(final_waits=finals)
    return nc


def build_router(P, nc, router, rt_t, ident_t, XNt, gate_bc, ps_rot, NTB):
    rt_v = router.rearrange("(kc p) e -> p kc e", p=128)
    P.dma("gpsimd", rt_t[:, :, :], rt_v, owner=rt_t, writes=[rt_t])
    P.op("gpsimd", lambda e: e.memset(ident_t[:, :], 0.0), writes=[ident_t])
    P.op("gpsimd", lambda e: e.affine_select(ident_t[:, :], ident_t[:, :], [[-1, 128]], ALU.not_equal, 1.0,
                                             base=0, channel_multiplier=1), reads=[ident_t], writes=[ident_t])
    lg = P.sbt([128, 8], F32, "lg")
    mx = P.sbt([128, 8], F32, "mx")
    dlt = P.sbt([128, 1], F32, "dlt")
    p0 = P.sbt([128, 1], F32, "p0")
    p1 = P.sbt([128, 1], F32, "p1")
    g0 = P.sbt([128, 8], F32, "g0")
    g1 = P.sbt([128, 8], F32, "g1")
    G = P.sbt([128, 128], BF16, "Gb")
    for n in range(NTB):
        for tt in range(4):
            pt = ps_rot.next()
            for kc in range(KC):
                P.op("tensor", lambda e, pt=pt, kc=kc, n=n, tt=tt: e.matmul(
                    pt[:, 0:8], XNt[n][:, kc, tt * 128:(tt + 1) * 128], rt_t[:, kc, :],
                    start=(kc == 0), stop=(kc == KC - 1)), reads=[XNt[n], rt_t], writes=[pt])
            P.op("vector", lambda e, pt=pt: e.tensor_copy(lg[:, :], pt[:, 0:8]), reads=[pt], writes=[lg])
            P.op("vector", lambda e: e.max(mx[:, :], lg[:, :]), reads=[lg], writes=[mx])
            P.op("vector", lambda e: e.tensor_tensor(dlt[:, :], mx[:, 0:1], mx[:, 1:2], ALU.subtract),
                 reads=[mx], writes=[dlt])
            P.op("scalar", lambda e: e.activation(p0[:, :], dlt[:, :], AF.Sigmoid), reads=[dlt], writes=[p0])
            P.op("scalar", lambda e: e.activation(p1[:, :], dlt[:, :], AF.Sigmoid, scale=-1.0), reads=[dlt], writes=[p1])
            P.op("vector", lambda e: e.tensor_scalar(g0[:, :], lg[:, :], mx[:, 0:1], p0[:, 0:1], ALU.is_equal, ALU.mult),
                 reads=[lg, mx, p0], writes=[g0])
            P.op("vector", lambda e: e.tensor_scalar(g1[:, :], lg[:, :], mx[:, 1:2], p1[:, 0:1], ALU.is_equal, ALU.mult),
                 reads=[lg, mx, p1], writes=[g1])
            P.op("vector", lambda e: e.tensor_tensor(g0[:, :], g0[:, :], g1[:, :], ALU.add), reads=[g0, g1], writes=[g0])
            for ex in range(8):
                P.op("vector", lambda e, ex=ex: e.tensor_copy(G[:, :], g0[:, ex:ex + 1].to_broadcast([128, 128])),
                     reads=[g0], writes=[G])
                pb = ps_rot.next()
                P.op("tensor", lambda e, pb=pb: e.matmul(pb[:, 0:128], G[:, :], ident_t[:, :], start=True, stop=True),
                     reads=[G, ident_t], writes=[pb])
                gb = gate_bc[ex][n]
                P.op("scalar", lambda e, gb=gb, pb=pb, tt=tt: e.copy(gb[:, tt * 128:(tt + 1) * 128], pb[:, 0:128]),
                     reads=[pb], writes=[gb])


L = 64
SEG = 512
NCH = SEG // L
NSEG = T // SEG


def bc_mid(ap2d, n):
    a = ap2d.ap
    return bass.AP(ap2d.tensor, ap2d.offset, [list(a[0]), [0, n], list(a[1])])


def bc_last(ap2d, n):
    a = ap2d.ap
    return bass.AP(ap2d.tensor, ap2d.offset, [list(a[0]), list(a[1]), [0, n]])


class PhaseA:
    def __init__(self, nc, stack):
        self.nc = nc
        self.P = Prog(nc, stack)
        P = self.P
        self.ps_rot = Rot([P.pst([128, 512], F32, f"psa{i}") for i in range(6)])
        self.psb_rot = Rot([P.pst([128, 512], BF16, f"psab{i}") for i in range(2)])
        self.XN_rot = Rot([P.sbt([128, KC, SEG], BF16, f"XN{i}") for i in range(2)])
        self.XNcur = None
        self.w_rot = Rot([P.sbt([128, KC, 256], BF16, f"wa{i}") for i in range(2)])
        self.consts()
        self.xs_rot = Rot([P.sbt([128, KC, 64], F32, f"xs{i}") for i in range(2)])
        self.sq_rot = Rot([P.sbt([128, 64], BF16, f"sqA{i}") for i in range(2)])
        self.rs = P.sbt([128, 64], F32, "rsA")
        self.nw = None

    def consts(self):
        P = self.P
        self.ones = P.sbt([128, 128], BF16, "onesA")
        P.op("vector", lambda e: e.memset(self.ones[:, :], 1.0), writes=[self.ones])
        self.onesf = P.sbt([128, 64], F32, "onesF")
        P.op("vector", lambda e: e.memset(self.onesf[:, :], 1.0), writes=[self.onesf])
        self.eps = P.sbt([128, 1], F32, "epsA")
        P.op("vector", lambda e: e.memset(self.eps[:, :], EPS), writes=[self.eps])
        self.ident = P.sbt([128, 128], BF16, "identA")
        P.op("gpsimd", lambda e: e.memset(self.ident[:, :], 0.0), writes=[self.ident])
        P.op("gpsimd", lambda e: e.affine_select(self.ident[:, :], self.ident[:, :], [[-1, 128]], ALU.not_equal, 1.0,
                                                 base=0, channel_multiplier=1), reads=[self.ident], writes=[self.ident])
        self.identf = P.sbt([64, 64], F32, "identF")
        P.op("gpsimd", lambda e: e.memset(self.identf[:, :], 0.0), writes=[self.identf])
        P.op("gpsimd", lambda e: e.affine_select(self.identf[:, :], self.identf[:, :], [[-1, 64]], ALU.not_equal, 1.0,
                                                 base=0, channel_multiplier=1), reads=[self.identf], writes=[self.identf])
        self.m_incl = P.sbt([64, 64], F32, "m_incl")
        self.m_strict = P.sbt([64, 64], F32, "m_strict")
        self.m_low = P.sbt([64, 64], F32, "m_low")
        for mt, op, cm, st in ((self.m_incl, ALU.is_ge, -1, 1), (self.m_strict, ALU.is_gt, -1, 1),
                               (self.m_low, ALU.is_gt, 1, -1)):
            P.op("gpsimd", lambda e, mt=mt: e.memset(mt[:, :], 1.0), writes=[mt])
            P.op("gpsimd", lambda e, mt=mt, op=op, cm=cm, st=st: e.affine_select(
                mt[:, :], mt[:, :], [[st, 64]], op, 0.0, base=0, channel_multiplier=cm), reads=[mt], writes=[mt])
        cm_h = P.sb([128, NCH, L], F32, "cmask")
        self.cmask = Tile(cm_h[:, :, :])
        P.op("vector", lambda e: e.memset(cm_h[:, :, :], 1.0), writes=[self.cmask])
        P.op("vector", lambda e: e.memset(cm_h[:, :, 0:1], 0.0), reads=[self.cmask], writes=[self.cmask])
        self.cmask2 = cm_h[:, :, :].rearrange("p c l -> p (c l)")

    def small(self, dram_ap, shape, name, dtype=F32, eng="sync"):
        t = self.P.sbt(shape, dtype, name + "_sb")
        idx = tuple(slice(None) for _ in shape)
        self.P.dma(eng if dtype == F32 else "gpsimd", t[idx], dram_ap, owner=t, writes=[t])
        return t

    def compute_xn(self, xT, norm_dram, seg):
        P = self.P
        if self.nw is None:
            self.nw = self.small(norm_dram, [128, KC], "nwA")
        nw = self.nw
        xv = xT.rearrange("(kc p) t -> p kc t", p=128)
        XN = self.XN_rot.next()
        self.XNcur = XN
        rs = self.rs
        for blk in range(SEG // 64):
            t0 = seg * SEG + blk * 64
            xs = self.xs_rot.next()
            for k0 in range(0, KC, 4):
                P.dma("sync", xs[:, k0:k0 + 4, :], xv[:, k0:k0 + 4, t0:t0 + 64], owner=xs, writes=[xs])
            pt = self.ps_rot.next()
            for kc in range(KC):
                sq = self.sq_rot.next()
                P.op("scalar", lambda e, sq=sq, xs=xs, kc=kc: e.activation(sq[:, :], xs[:, kc, :], AF.Square),
                     reads=[xs], writes=[sq])
                P.op("tensor", lambda e, pt=pt, sq=sq, kc=kc: e.matmul(
                    pt[:, 0:64], self.ones[:, :], sq[:, :], start=(kc == 0), stop=(kc == KC - 1)),
                    reads=[self.ones, sq], writes=[pt])
            rsqrt_op(P, rs, rs[:, :], pt, pt[:, 0:64], 1.0 / D, self.eps)
            for kc in range(KC):
                P.op("vector", lambda e, kc=kc, xs=xs, blk=blk, XN=XN: e.scalar_tensor_tensor(
                    XN[:, kc, blk * 64:(blk + 1) * 64], xs[:, kc, :], nw[:, kc:kc + 1], rs[:, :], ALU.mult, ALU.mult),
                    reads=[xs, nw, rs, XN], writes=[XN])

    def load_w(self, dram2d, ncols):
        wt = self.w_rot.next()
        v = dram2d.rearrange("(kc p) c -> p kc c", p=128)
        for k0 in range(0, KC, 8):
            self.P.dma("gpsimd", wt[:, k0:k0 + 8, 0:ncols], v[:, k0:k0 + 8, :], owner=wt, writes=[wt])
        return wt

    def proj_cm(self, pt, wt, c0, ncols, seg):
        XN = self.XNcur
        for kc in range(KC):
            self.P.op("tensor", lambda e, kc=kc: e.matmul(
                pt[0:ncols, 0:SEG], wt[:, kc, c0:c0 + ncols], XN[:, kc, :],
                start=(kc == 0), stop=(kc == KC - 1)), reads=[wt, XN], writes=[pt])

    def proj_tm(self, pt_ap, pt, wt, c0, ncols, seg, t0, nt):
        XN = self.XNcur
        for kc in range(KC):
            self.P.op("tensor", lambda e, kc=kc: e.matmul(
                pt_ap, XN[:, kc, t0:t0 + nt], wt[:, kc, c0:c0 + ncols],
                start=(kc == 0), stop=(kc == KC - 1)), reads=[wt, XN], writes=[pt])


DBG_ONLY = ""
DBG_STAGE = 0
RW_DS = math.exp(-0.5)
RW_LN_EPS = 64e-5


def v3(ap, c=NCH):
    return ap.rearrange("p (c l) -> p c l", c=c)


def build_phase_a0():
    nc = bass.Bass("TRN2", target_bir_lowering=False)
    dr = lambda name, shape, dt=F32: nc.dram_tensor(name, list(shape), dt, kind="ExternalInput").ap()
    xT = dr("xT", [D, T]); norm1 = dr("norm1", [128, KC])
    w_rkv = dr("w_rkv", [D, 1536]); w_lora = dr("w_lora", [D, 448])
    mu_rkv = dr("mu_rkv", [64, 24]); mu_l = dr("mu_l", [128, 4])
    rwp = dr("rwp", [64, 5, 8])
    w2 = dr("w2", [96, 512]); a2 = dr("a2", [96, 512]); g2 = dr("g2", [256, 512])
    lnw = dr("lnw", [64, 512]); lnb = dr("lnb", [64, 512])
    w_qk = dr("w_qk", [D, 512]); w_v = dr("w_v", [D, 512]); w_o = dr("w_o", [D, 512]); w_if = dr("w_if", [D, 4])
    cw = dr("cw", [128, 4, 4]); cb = dr("cb", [128, 4])
    ifb_c = dr("ifb_c", [4, 1]); ifb_t = dr("ifb_t", [64, 4]); mnw = dr("mnw", [64, 512])
    y_out = nc.dram_tensor("y_tok", [T, 1024], F32, kind="ExternalOutput").ap()

    with ExitStack() as stack:
        A = PhaseA(nc, stack)
        P = A.P
        finals = []
        mu_rkv_t = A.small(mu_rkv, [64, 24], "mu_rkv"); mu_l_t = A.small(mu_l, [128, 4], "mu_l")
        rwp_t = A.small(rwp, [64, 5, 8], "rwp")
        w2_t = A.small(w2, [96, 512], "w2", BF16); a2_t = A.small(a2, [96, 512], "a2", BF16)
        g2_t = A.small(g2.rearrange("(kb p) c -> p kb c", p=128), [128, 2, 512], "g2", BF16)
        lnw_t = A.small(lnw, [64, 512], "lnw"); lnb_t = A.small(lnb, [64, 512], "lnb")
        cw_t = A.small(cw, [128, 4, 4], "cw"); cb_t = A.small(cb, [128, 4], "cb")
        ifb_c_t = A.small(ifb_c, [4, 1], "ifb_c"); ifb_t_t = A.small(ifb_t, [64, 4], "ifb_t")
        mnw_t = A.small(mnw, [64, 512], "mnw")
        omka = P.sbt([64, 8], F32, "omka")
        P.op("vector", lambda e: e.tensor_scalar(omka[:, :], rwp_t[:, 3, :], -1.0, 1.0, ALU.mult, ALU.add),
             reads=[rwp_t], writes=[omka])
        lneps = P.sbt([64, 1], F32, "lneps")
        P.op("vector", lambda e: e.memset(lneps[:, :], RW_LN_EPS), writes=[lneps])
        M2 = P.sbt([64, 128], F32, "M2")
        P.op("vector", lambda e: e.tensor_copy(M2[:, 0:64], A.m_strict[:, :]), reads=[A.m_strict], writes=[M2])
        P.op("vector", lambda e: e.tensor_copy(M2[:, 64:128], A.m_incl[:, :]), reads=[A.m_incl, M2], writes=[M2])
        ID8 = P.sbt([64, SEG], F32, "ID8")
        P.op("vector", lambda e: e.tensor_copy(v3(ID8[:, :]), bc_mid(A.identf[:, :], NCH)), reads=[A.identf], writes=[ID8])
        Sel = P.sbt([4, 4, 128], F32, "Sel")
        for h in range(4):
            P.op("vector", lambda e, h=h: e.tensor_copy(Sel[0:4, h, :], A.identf[0:4, h:h + 1].to_broadcast([4, 128])),
                 reads=[A.identf, Sel], writes=[Sel])

        def T2(shape, dt, name, n=2):
            return Rot([P.sbt(shape, dt, f"{name}{i}") for i in range(n)])

        def act(out_t, out_ap, in_t, in_ap, func, **kw):
            rd = [in_t] + [kw.pop("bias_t")] if "bias_t" in kw else [in_t]
            P.op("scalar", lambda e: e.activation(out_ap, in_ap, func, **kw), reads=rd + [out_t], writes=[out_t])

        def tt(eng, out_t, out_ap, a_t, a_ap, b_t, b_ap, op):
            P.op(eng, lambda e: e.tensor_tensor(out_ap, a_ap, b_ap, op), reads=[a_t, b_t, out_t], writes=[out_t])

        def ts(eng, out_t, out_ap, a_t, a_ap, s1, s2, op0, op1=None, extra=()):
            if op1 is None:
                P.op(eng, lambda e: e.tensor_scalar(out_ap, a_ap, s1, None, op0), reads=[a_t, out_t] + list(extra), writes=[out_t])
            else:
                P.op(eng, lambda e: e.tensor_scalar(out_ap, a_ap, s1, s2, op0, op1), reads=[a_t, out_t] + list(extra), writes=[out_t])

        def stt(eng, out_t, out_ap, a_t, a_ap, sc, b_t, b_ap, op0, op1, extra=()):
            P.op(eng, lambda e: e.scalar_tensor_tensor(out_ap, a_ap, sc, b_ap, op0, op1),
                 reads=[a_t, b_t, out_t] + list(extra), writes=[out_t])

        def mm(pt, out_ap, l_t, l_ap, r_t, r_ap, start=True, stop=True):
            P.op("tensor", lambda e: e.matmul(out_ap, l_ap, r_ap, start=start, stop=stop), reads=[l_t, r_t], writes=[pt])

        halo = P.sbt([64, 24], F32, "halo")
        halo_l = P.sbt([128, 4], F32, "halo_l")
        P.op("vector", lambda e: e.memset(halo[:, :], 0.0), writes=[halo])
        P.op("vector", lambda e: e.memset(halo_l[:, :], 0.0), writes=[halo_l])
        S_f = [P.sbt([64, 64], F32, f"S{h}") for h in range(8)]
        S_b = [P.sbt([64, 64], BF16, f"Sb{h}") for h in range(8)]
        for h in range(8):
            P.op("gpsimd", lambda e, h=h: e.memset(S_f[h][:, :], 0.0), writes=[S_f[h]])
            P.op("gpsimd", lambda e, h=h: e.memset(S_b[h][:, :], 0.0), writes=[S_b[h]])
        raw_rot = T2([128, 3 + SEG], F32, "raw", 3)
        f32r = {k: T2([64, SEG], F32, k, 1) for k in ("rf", "kf", "vf", "lw", "aa", "kk", "kkn", "k2", "bb", "eb", "enb", "ebm", "tmp", "tmp2", "dnl")}
        sqr = T2([64, SEG], BF16, "sqr", 1); vbf_r = T2([64, SEG], BF16, "vbf", 1); khat_r = T2([64, SEG], BF16, "khat", 1); bhat_r = T2([64, SEG], BF16, "bhat", 1)
        rkr_r = T2([64, SEG], BF16, "rkr", 1)
        tdw = P.sbt([96, SEG], BF16, "tdw"); daf = P.sbt([96, SEG], BF16, "daf"); sdg = P.sbt([128, 2, SEG], BF16, "sdg")
        AR_r = T2([64, NCH, 2, L], BF16, "AR", 1); KB_r = T2([64, NCH, 2, L], BF16, "KB", 1)
        KT_r = T2([64, NCH, L], BF16, "KT", 1); BT_r = T2([64, NCH, L], BF16, "BT", 1); VT_r = T2([64, NCH, L], BF16, "VT", 1)
        AKRK_r = T2([64, NCH, 128], BF16, "AKRK", 1); ABRB_r = T2([64, NCH, 128], BF16, "ABRB", 1); NT_r = T2([64, NCH, L], BF16, "NT", 1)
        Pa_r = T2([64, NCH, L], BF16, "Pa", 2); Pt_r = T2([64, NCH, L], BF16, "Ptt", 2); X_r = T2([64, NCH, L], BF16, "Xi", 1)
        Pbf_r = T2([64, L], BF16, "Pbf", 3); Ubf_r = T2([64, L], BF16, "Ubf", 3)
        Y_r = T2([64, NCH, L], F32, "Yr", 1); yc_r = T2([64, NCH, L], F32, "ycr", 1); st_r = T2([64, NCH], F32, "str", 4)
        yo_r = T2([64, NCH, L], F32, "yor", 1)

        def shift_mix(pt, npart, halo_t, halo_ap, mu_ap, mu_t, out_t, out_ap):
            raw = raw_rot.next()
            P.op("vector", lambda e: e.tensor_copy(raw[0:npart, 2:3], halo_ap), reads=[halo_t, raw], writes=[raw])
            P.op("scalar", lambda e: e.copy(raw[0:npart, 3:3 + SEG], pt[0:npart, 0:SEG]), reads=[pt, raw], writes=[raw])
            P.op("vector", lambda e: e.tensor_copy(halo_ap, raw[0:npart, 2 + SEG:3 + SEG]), reads=[raw, halo_t], writes=[halo_t])
            tmp = f32r["tmp"].next() if npart <= 64 else None
            if tmp is None:
                tmp = raw_rot.next()
                tap = tmp[0:npart, 0:SEG]
            else:
                tap = tmp[0:npart, :]
            tt("vector", tmp, tap, raw, raw[0:npart, 2:2 + SEG], raw, raw[0:npart, 3:3 + SEG], ALU.subtract)
            stt("vector", out_t, out_ap, tmp, tap, mu_ap, raw, raw[0:npart, 3:3 + SEG], ALU.mult, ALU.add, extra=[mu_t])

        ml_seg = build_mlstm(A, dict(w_qk=w_qk, w_v=w_v, w_o=w_o, w_if=w_if, cw_t=cw_t, cb_t=cb_t, ifb_c_t=ifb_c_t, ifb_t_t=ifb_t_t,
                                     mnw_t=mnw_t, Sel=Sel, ID8=ID8, scr=[f32r[k].tiles[0] for k in ("rf", "kf", "vf", "lw", "aa", "kk")]), y_out, finals)
        for seg in range(NSEG):
            A.compute_xn(xT, norm1, seg)
            ml_seg(seg)
            wl = None
            dwf = f32r["tmp2"].next()
            for (c0, ncol, hi, kind) in ((0, 96, 0, "dw"), (96, 96, 1, "da"), (192, 128, 2, "dg0"), (320, 128, 3, "dg1")):
                pt = A.ps_rot.next()
                if kind in ("dw", "dg0"):
                    wl = A.load_w(w_lora[:, c0:c0 + 192 if kind == "dw" else c0 + 256], 192 if kind == "dw" else 256)
                    wl_c0 = c0
                A.proj_cm(pt, wl, c0 - wl_c0, ncol, seg)
                mix = raw_rot.next()
                shift_mix(pt, ncol, halo_l, halo_l[0:ncol, hi:hi + 1], mu_l_t[0:ncol, hi:hi + 1], mu_l_t, mix, mix[0:ncol, 0:SEG])
                if kind == "dw":
                    act(tdw, tdw[:, :], mix, mix[0:96, 0:SEG], AF.Tanh)
                elif kind == "da":
                    P.op("vector", lambda e, mix=mix: e.tensor_copy(daf[:, :], mix[0:96, 0:SEG]), reads=[mix, daf], writes=[daf])
                else:
                    kb = 0 if kind == "dg0" else 1
                    act(sdg, sdg[:, kb, :], mix, mix[0:128, 0:SEG], AF.Sigmoid)
            for h in range(8):
                wr = A.load_w(w_rkv[:, h * 192:(h + 1) * 192], 192)
                q3 = {}
                for qi, nm in enumerate(("rf", "kf", "vf")):
                    pt = A.ps_rot.next()
                    A.proj_cm(pt, wr, qi * 64, 64, seg)
                    o = f32r[nm].next()
                    shift_mix(pt, 64, halo, halo[:, h * 3 + qi:h * 3 + qi + 1], mu_rkv_t[:, h * 3 + qi:h * 3 + qi + 1], mu_rkv_t, o, o[:, :])
                    q3[nm] = o
                rf, kf, vf = q3["rf"], q3["kf"], q3["vf"]
                hc = slice(h * 64, (h + 1) * 64)
                pt = A.ps_rot.next()
                mm(pt, pt[0:64, 0:SEG], w2_t, w2_t[:, hc], tdw, tdw[:, :])
                lw = f32r["lw"].next()
                act(lw, lw[:, :], pt, pt[0:64, 0:SEG], AF.Sigmoid, bias=rwp_t[:, 0, h:h + 1], bias_t=rwp_t)
                ts("vector", lw, lw[:, :], lw, lw[:, :], -RW_DS, None, ALU.mult)
                pt = A.ps_rot.next()
                mm(pt, pt[0:64, 0:SEG], a2_t, a2_t[:, hc], daf, daf[:, :])
                aa = f32r["aa"].next()
                act(aa, aa[:, :], pt, pt[0:64, 0:SEG], AF.Sigmoid, bias=rwp_t[:, 1, h:h + 1], bias_t=rwp_t)
                kk = f32r["kk"].next()
                ts("vector", kk, kk[:, :], kf, kf[:, :], rwp_t[:, 2, h:h + 1], None, ALU.mult, extra=[rwp_t])
                sq = sqr.next()
                act(sq, sq[:, :], kk, kk[:, :], AF.Square)
                pt = A.ps_rot.next()
                mm(pt, pt[0:64, 0:SEG], A.ones, A.ones[0:64, 0:64], sq, sq[:, :])
                kkn = f32r["kkn"].next()
                ts("vector", kkn, kkn[:, :], pt, pt[0:64, 0:SEG], 1e-12, None, ALU.max)
                act(kkn, kkn[:, :], kkn, kkn[:, :], AF.Sqrt)
                P.op("vector", lambda e, kkn=kkn: e.reciprocal(kkn[:, :], kkn[:, :]), reads=[kkn], writes=[kkn])
                tt("vector", kkn, kkn[:, :], kkn, kkn[:, :], kk, kk[:, :], ALU.mult)
                k2 = f32r["k2"].next()
                ts("vector", k2, k2[:, :], aa, aa[:, :], rwp_t[:, 3, h:h + 1], omka[:, h:h + 1], ALU.mult, ALU.add, extra=[rwp_t, omka])
                tt("vector", k2, k2[:, :], k2, k2[:, :], kf, kf[:, :], ALU.mult)
                bb = f32r["bb"].next()
                P.op("vector", lambda e, bb=bb, lw=lw: e.tensor_tensor_scan(bb[:, :], A.cmask2[0:64, :], lw[:, :], 0.0, ALU.mult, ALU.add),
                     reads=[A.cmask, lw, bb], writes=[bb])
                eb = f32r["eb"].next(); enb = f32r["enb"].next(); ebm = f32r["ebm"].next(); dnl = f32r["dnl"].next()
                act(eb, eb[:, :], bb, bb[:, :], AF.Exp)
                act(enb, enb[:, :], bb, bb[:, :], AF.Exp, scale=-1.0)
                tt("gpsimd", ebm, ebm[:, :], bb, bb[:, :], lw, lw[:, :], ALU.subtract)
                act(ebm, ebm[:, :], ebm, ebm[:, :], AF.Exp)
                tt("gpsimd", dnl, v3(dnl[:, :]), bb, v3(bb[:, :]), bb, bc_last(bb[:, L - 1:SEG:L], L), ALU.subtract)
                act(dnl, dnl[:, :], dnl, dnl[:, :], AF.Exp, scale=-1.0)
                AR = AR_r.next(); KB = KB_r.next()
                stt("vector", AR, AR[:, :, 0, :], kkn, v3(kkn[:, :]), -1.0, ebm, v3(ebm[:, :]), ALU.mult, ALU.mult)
                tt("vector", AR, AR[:, :, 1, :], rf, v3(rf[:, :]), eb, v3(eb[:, :]), ALU.mult)
                tt("vector", KB, KB[:, :, 0, :], k2, v3(k2[:, :]), enb, v3(enb[:, :]), ALU.mult)
                tmp2 = f32r["tmp2"].next()
                tt("gpsimd", tmp2, tmp2[:, :], kkn, kkn[:, :], aa, aa[:, :], ALU.mult)
                tt("vector", KB, KB[:, :, 1, :], tmp2, v3(tmp2[:, :]), enb, v3(enb[:, :]), ALU.mult)
                khat = khat_r.next(); bhat = bhat_r.next(); vbf = vbf_r.next()
                tt("vector", khat, khat[:, :], k2, k2[:, :], dnl, dnl[:, :], ALU.mult)
                tt("vector", bhat, bhat[:, :], tmp2, tmp2[:, :], dnl, dnl[:, :], ALU.mult)
                P.op("scalar", lambda e, vbf=vbf, vf=vf: e.copy(vbf[:, :], vf[:, :]), reads=[vf, vbf], writes=[vbf])
                KT = KT_r.next(); BT = BT_r.next(); VT = VT_r.next()
                for src, dst in ((khat, KT), (bhat, BT), (vbf, VT)):
                    pb = A.psb_rot.next()
                    for c in range(NCH):
                        P.op("tensor", lambda e, pb=pb, src=src, c=c: e.transpose(
                            pb[0:64, c * L:(c + 1) * L], src[:, c * L:(c + 1) * L], A.ident[0:64, 0:64]),
                            reads=[src, A.ident], writes=[pb])
                    P.op("scalar", lambda e, pb=pb, dst=dst: e.copy(dst[:, :, :], v3(pb[0:64, 0:SEG])), reads=[pb, dst], writes=[dst])
                AKRK = AKRK_r.next(); ABRB = ABRB_r.next(); NT = NT_r.next()
                for which, dst in ((0, AKRK), (1, ABRB)):
                    for cg in range(2):
                        pt = A.ps_rot.next()
                        for cc in range(4):
                            c = cg * 4 + cc
                            mm(pt, pt[0:64, cc * 128:(cc + 1) * 128], KB, KB[:, c, which, :], AR, AR[:, c, :, :].rearrange("p a l -> p (a l)"))
                        tt("vector", dst, dst[:, cg * 4:(cg + 1) * 4, :], pt, pt[0:64, 0:512].rearrange("p (c l) -> p c l", c=4), M2, bc_mid(M2[:, :], 4), ALU.mult)
                pt = A.ps_rot.next()
                for c in range(NCH):
                    mm(pt, pt[0:64, c * L:(c + 1) * L], AR, AR[:, c, 0, :], KB, KB[:, c, 1, :])
                tt("vector", NT, NT[:, :, :], pt, v3(pt[0:64, 0:SEG]), A.m_low, bc_mid(A.m_low[:, :], NCH), ALU.mult)
                X = X_r.next()
                tt("gpsimd", X, X[:, :, :], ABRB, ABRB[:, :, 0:64], A.ident, bc_mid(A.ident[0:64, 0:64], NCH), ALU.add)
                Pk_t, Pk = ABRB, (lambda c: ABRB[:, c, 0:64])
                Ptk_t, Ptk = NT, (lambda c: NT[:, c, :])
                for lev in range(1, 6):
                    if lev < 5:
                        nPk = Pa_r.next()
                        pt = A.ps_rot.next()
                        for c in range(NCH):
                            mm(pt, pt[0:64, c * L:(c + 1) * L], Ptk_t, Ptk(c), Pk_t, Pk(c))
                        P.op("scalar", lambda e, pt=pt, nPk=nPk: e.copy(nPk[:, :, :], v3(pt[0:64, 0:SEG])), reads=[pt, nPk], writes=[nPk])
                    nPt = Pt_r.next()
                    pt = A.ps_rot.next()
                    for c in range(NCH):
                        mm(pt, pt[0:64, c * L:(c + 1) * L], Pk_t, Pk(c), Ptk_t, Ptk(c))
                    P.op("scalar", lambda e, pt=pt, nPt=nPt: e.copy(nPt[:, :, :], v3(pt[0:64, 0:SEG])), reads=[pt, nPt], writes=[nPt])
                    pt = A.ps_rot.next()
                    for c in range(NCH):
                        mm(pt, pt[0:64, c * L:(c + 1) * L], nPt, nPt[:, c, :], X, X[:, c, :])
                    tt("vector", X, X[:, :, :], X, X[:, :, :], pt, v3(pt[0:64, 0:SEG]), ALU.add)
                    if lev < 5:
                        Pk_t, Pk = nPk, (lambda c, nPk=nPk: nPk[:, c, :])
                    Ptk_t, Ptk = nPt, (lambda c, nPt=nPt: nPt[:, c, :])
                Y = Y_r.next()
                for c in range(NCH):
                    pa = A.ps_rot.next()
                    mm(pa, pa[0:64, 0:64], AR, AR[:, c, 0, :], S_b[h], S_b[h][:, :], True, False)
                    mm(pa, pa[0:64, 0:64], AKRK, AKRK[:, c, 0:64], VT, VT[:, c, :], False, True)
                    Pbf = Pbf_r.next()
                    P.op("vector", lambda e, pa=pa, Pbf=Pbf: e.tensor_copy(Pbf[:, :], pa[0:64, 0:64]), reads=[pa, Pbf], writes=[Pbf])
                    pb_ = A.ps_rot.next()
                    mm(pb_, pb_[0:64, 0:64], X, X[:, c, :], Pbf, Pbf[:, :])
                    Ubf = Ubf_r.next()
                    P.op("vector", lambda e, pb_=pb_, Ubf=Ubf: e.tensor_copy(Ubf[:, :], pb_[0:64, 0:64]), reads=[pb_, Ubf], writes=[Ubf])
                    pd = A.ps_rot.next()
                    mm(pd, pd[0:64, 0:64], KT, KT[:, c, :], VT, VT[:, c, :], True, False)
                    mm(pd, pd[0:64, 0:64], BT, BT[:, c, :], Ubf, Ubf[:, :], False, True)
                    pc = A.ps_rot.next()
                    mm(pc, pc[0:64, 0:64], AR, AR[:, c, 1, :], S_b[h], S_b[h][:, :], True, False)
                    mm(pc, pc[0:64, 0:64], AKRK, AKRK[:, c, 64:128], VT, VT[:, c, :], False, False)
                    mm(pc, pc[0:64, 0:64], ABRB, ABRB[:, c, 64:128], Ubf, Ubf[:, :], False, True)
                    P.op("scalar", lambda e, pc=pc, Y=Y, c=c: e.copy(Y[:, c, :], pc[0:64, 0:64]), reads=[pc, Y], writes=[Y])
                    stt("vector", S_f[h], S_f[h][:, :], S_f[h], S_f[h][:, :], eb[:, c * L + L - 1:c * L + L], pd, pd[0:64, 0:64], ALU.mult, ALU.add, extra=[eb])
                    P.op("scalar", lambda e, h=h: e.copy(S_b[h][:, :], S_f[h][:, :]), reads=[S_f[h], S_b[h]], writes=[S_b[h]])
                st = st_r.next(); yc = yc_r.next()
                P.op("vector", lambda e, st=st, Y=Y: e.tensor_reduce(st[:, :], Y[:, :, :], AX.X, ALU.add), reads=[Y, st], writes=[st])
                ts("vector", st, st[:, :], st, st[:, :], 1.0 / L, None, ALU.mult)
                tt("vector", yc, yc[:, :, :], Y, Y[:, :, :], st, bc_last(st[:, :], L), ALU.subtract)
                ysq = Y
                tt("gpsimd", ysq, ysq[:, :, :], yc, yc[:, :, :], yc, yc[:, :, :], ALU.mult)
                st2 = st_r.next()
                P.op("vector", lambda e, st2=st2, ysq=ysq: e.tensor_reduce(st2[:, :], ysq[:, :, :], AX.X, ALU.add), reads=[ysq, st2], writes=[st2])
                act(st2, st2[:, :], st2, st2[:, :], AF.Sqrt, bias=lneps[:, 0:1], scale=1.0 / L, bias_t=lneps)
                P.op("vector", lambda e, st2=st2: e.reciprocal(st2[:, :], st2[:, :]), reads=[st2], writes=[st2])
                tt("vector", yc, yc[:, :, :], yc, yc[:, :, :], st2, bc_last(st2[:, :], L), ALU.mult)
                tt("vector", yc, yc[:, :, :], yc, yc[:, :, :], lnw_t, bc_mid(lnw_t[:, hc], NCH), ALU.mult)
                tt("vector", yc, yc[:, :, :], yc, yc[:, :, :], lnb_t, bc_mid(lnb_t[:, hc], NCH), ALU.add)
                rkr = rkr_r.next()
                stt("vector", rkr, rkr[:, :], rf, rf[:, :], rwp_t[:, 4, h:h + 1], k2, k2[:, :], ALU.mult, ALU.mult, extra=[rwp_t])
                pq = A.ps_rot.next()
                for c in range(NCH):
                    mm(pq, pq[0:64, c:c + 1], rkr, rkr[:, c * L:(c + 1) * L], A.ones, A.ones[0:64, 0:1])
                bon = st_r.next()
                P.op("vector", lambda e, bon=bon, pq=pq: e.tensor_copy(bon[:, :], pq[0:64, 0:NCH]), reads=[pq, bon], writes=[bon])
                tt("vector", ysq, ysq[:, :, :], VT, VT[:, :, :], bon, bc_last(bon[:, :], L), ALU.mult)
                tt("vector", yc, yc[:, :, :], yc, yc[:, :, :], ysq, ysq[:, :, :], ALU.add)
                pg = A.ps_rot.next()
                for c in range(NCH):
                    for kb in range(2):
                        mm(pg, pg[0:64, c * L:(c + 1) * L], sdg, sdg[:, kb, c * L:(c + 1) * L], g2_t, g2_t[:, kb, hc], kb == 0, kb == 1)
                yo = yo_r.next()
                tt("vector", yo, yo[:, :, :], yc, yc[:, :, :], pg, v3(pg[0:64, 0:SEG]), ALU.mult)
                finals.append(P.dma("sync", y_out[seg * SEG:(seg + 1) * SEG, hc].rearrange("(c t) i -> t c i", c=NCH),
                                    yo[:, :, :], owner=yo, reads=[yo]))
        P.emit(final_waits=finals)
    return nc


def build_mlstm(A, d, y_out, finals):
    P = A.P
    DK, DV = 128, 256

    def T2(shape, dt, name, n=2):
        return Rot([P.sbt(shape, dt, f"{name}{i}") for i in range(n)])

    def mm(pt, out_ap, l_t, l_ap, r_t, r_ap, start=True, stop=True):
        P.op("tensor", lambda e: e.matmul(out_ap, l_ap, r_ap, start=start, stop=stop), reads=[l_t, r_t], writes=[pt])

    cw_t, cb_t, Sel, ID8 = d["cw_t"], d["cb_t"], d["Sel"], d["ID8"]
    halo = P.sbt([128, 4, 3], F32, "mhalo")
    P.op("vector", lambda e: e.memset(halo[:, :, :], 0.0), writes=[halo])
    C_f = [P.sbt([128, DV + 1], F32, f"Cf{h}") for h in range(2)]
    C_b = [P.sbt([128, DV + 1], BF16, f"Cb{h}") for h in range(2)]
    for h in range(2):
        P.op("gpsimd", lambda e, h=h: e.memset(C_f[h][:, :], 0.0), writes=[C_f[h]])
        P.op("gpsimd", lambda e, h=h: e.memset(C_b[h][:, :], 0.0), writes=[C_b[h]])
    raw_r = T2([128, 3 + SEG], F32, "mraw"); acc_r = T2([128, SEG], F32, "macc")
    qk_bf = [P.sbt([128, SEG], BF16, f"mqk{i}") for i in range(4)]
    qt_bf = [P.sbt([128, SEG], BF16, f"mqt{i}") for i in range(2)]
    Vp = P.sbt([64, NCH, 2, DV + 1], BF16, "mVp")
    P.op("vector", lambda e: e.memset(Vp[:, :, :, :], 1.0), writes=[Vp])
    og = P.sbt([64, NCH, 512], BF16, "mog")
    dch, dz, d1, pcum = d["scr"][0:4]
    EI = P.sbt([64, NCH, 4], F32, "mEI")
    E_t = d["scr"][4:6]
    PL = [P.sbt([128, NCH], F32, f"mPL{h}") for h in range(2)]
    KT = [P.sbt([64, NCH, DK], BF16, f"mKT{h}") for h in range(2)]
    W_r = T2([64, L], BF16, "mW", 3)
    H_r = T2([64, NCH, DV + 1], F32, "mH", 1)
    hs_r = T2([64, NCH, DV], F32, "mhs", 1)
    st_r = T2([64, NCH], F32, "mst", 4)

    def run_seg(seg):
            for blk in range(4):
                if blk % 2 == 0:
                    wqk = A.load_w(d["w_qk"][:, blk * 128:blk * 128 + 256], 256)
                pt = A.ps_rot.next()
                A.proj_cm(pt, wqk, (blk % 2) * 128, 128, seg)
                raw = raw_r.next()
                P.op("vector", lambda e, raw=raw, blk=blk: e.tensor_copy(raw[:, 0:3], halo[:, blk, :]), reads=[halo, raw], writes=[raw])
                P.op("scalar", lambda e, raw=raw, pt=pt: e.copy(raw[:, 3:3 + SEG], pt[:, 0:SEG]), reads=[pt, raw], writes=[raw])
                P.op("vector", lambda e, raw=raw, blk=blk: e.tensor_copy(halo[:, blk, :], raw[:, SEG:SEG + 3]), reads=[raw, halo], writes=[halo])
                acc = acc_r.next()
                P.op("vector", lambda e, acc=acc, raw=raw, blk=blk: e.tensor_scalar(
                    acc[:, :], raw[:, 3:3 + SEG], cw_t[:, blk, 3:4], cb_t[:, blk:blk + 1], ALU.mult, ALU.add),
                    reads=[raw, cw_t, cb_t, acc], writes=[acc])
                for tap in range(3):
                    P.op("vector", lambda e, acc=acc, raw=raw, blk=blk, tap=tap: e.scalar_tensor_tensor(
                        acc[:, :], raw[:, tap:tap + SEG], cw_t[:, blk, tap:tap + 1], acc[:, :], ALU.mult, ALU.add),
                        reads=[raw, cw_t, acc], writes=[acc])
                P.op("scalar", lambda e, acc=acc: e.activation(acc[:, :], acc[:, :], AF.Silu), reads=[acc], writes=[acc])
                if blk < 2:
                    P.op("vector", lambda e, acc=acc, blk=blk: e.tensor_scalar(qk_bf[blk][:, :], acc[:, :], DK ** -0.5, None, ALU.mult),
                         reads=[acc, qk_bf[blk]], writes=[qk_bf[blk]])
                else:
                    P.op("vector", lambda e, acc=acc, blk=blk: e.tensor_copy(qk_bf[blk][:, :], acc[:, :]), reads=[acc, qk_bf[blk]], writes=[qk_bf[blk]])
            wif = A.load_w(d["w_if"], 4)
            pt = A.ps_rot.next()
            A.proj_cm(pt, wif, 0, 4, seg)
            P.op("scalar", lambda e, pt=pt: e.activation(dch[0:4, :], pt[0:4, 0:SEG], AF.Sigmoid, bias=d["ifb_c_t"][:, 0:1]),
                 reads=[pt, d["ifb_c_t"], dch], writes=[dch])
            P.op("vector", lambda e: e.tensor_tensor(dz[0:4, :], dch[0:4, :], A.cmask2[0:4, :], ALU.mult), reads=[dch, A.cmask, dz], writes=[dz])
            P.op("vector", lambda e: e.tensor_tensor(d1[0:4, :], dch[0:4, :], dz[0:4, :], ALU.subtract), reads=[dch, dz, d1], writes=[d1])
            P.op("vector", lambda e: e.tensor_tensor_scan(pcum[0:4, :], dz[0:4, :], d1[0:4, :], 0.0, ALU.mult, ALU.add), reads=[dz, d1, pcum], writes=[pcum])
            for c in range(NCH):
                pt = A.ps_rot.next()
                A.proj_tm(pt[0:64, 0:4], pt, wif, 0, 4, seg, c * L, L)
                P.op("vector", lambda e, pt=pt, c=c: e.tensor_tensor(EI[:, c, :], pt[0:64, 0:4], d["ifb_t_t"][:, :], ALU.add),
                     reads=[pt, d["ifb_t_t"], EI], writes=[EI])
            P.op("scalar", lambda e: e.activation(EI[:, :, :], EI[:, :, :], AF.Exp), reads=[EI], writes=[EI])
            for hh in range(2):
                wv = A.load_w(d["w_v"][:, hh * 256:(hh + 1) * 256], 256)
                for c in range(NCH):
                    pt = A.ps_rot.next()
                    A.proj_tm(pt[0:64, 0:256], pt, wv, 0, 256, seg, c * L, L)
                    P.op("scalar", lambda e, pt=pt, c=c, hh=hh: e.copy(Vp[:, c, hh, 0:DV], pt[0:64, 0:256]), reads=[pt, Vp], writes=[Vp])
                wo = A.load_w(d["w_o"][:, hh * 256:(hh + 1) * 256], 256)
                for c in range(NCH):
                    pt = A.ps_rot.next()
                    A.proj_tm(pt[0:64, 0:256], pt, wo, 0, 256, seg, c * L, L)
                    P.op("scalar", lambda e, pt=pt, c=c, hh=hh: e.activation(og[:, c, hh * 256:(hh + 1) * 256], pt[0:64, 0:256], AF.Sigmoid),
                         reads=[pt, og], writes=[og])
            for h in range(2):
                pt = A.ps_rot.next()
                mm(pt, pt[0:64, 0:SEG], Sel, Sel[0:4, 2 + h, 0:64], dz, dz[0:4, :])
                P.op("vector", lambda e, pt=pt, h=h: e.tensor_tensor_scan(E_t[h][:, :], pt[0:64, 0:SEG], ID8[:, :], 0.0, ALU.mult, ALU.add),
                     reads=[pt, ID8, E_t[h]], writes=[E_t[h]])
                pt = A.ps_rot.next()
                mm(pt, pt[:, 0:SEG], Sel, Sel[0:4, 2 + h, :], pcum, pcum[0:4, :])
                P.op("vector", lambda e, pt=pt, h=h: e.tensor_tensor(qt_bf[h][:, :], qk_bf[h][:, :], pt[:, 0:SEG], ALU.mult),
                     reads=[pt, qk_bf[h], qt_bf[h]], writes=[qt_bf[h]])
                P.op("vector", lambda e, pt=pt, h=h: e.tensor_copy(PL[h][:, :], pt[:, L - 1:SEG:L]), reads=[pt, PL[h]], writes=[PL[h]])
                kbf = qk_bf[2 + h]
                for cg in range(2):
                    pb = A.psb_rot.next()
                    for cc in range(4):
                        c = cg * 4 + cc
                        P.op("tensor", lambda e, pb=pb, c=c, cc=cc, kbf=kbf: e.transpose(
                            pb[0:64, cc * 128:(cc + 1) * 128], kbf[:, c * L:(c + 1) * L], A.ident[:, :]), reads=[kbf, A.ident], writes=[pb])
                    for cc in range(4):
                        c = cg * 4 + cc
                        P.op("vector", lambda e, pb=pb, c=c, cc=cc, h=h: e.tensor_scalar(
                            KT[h][:, c, :], pb[0:64, cc * 128:(cc + 1) * 128], E_t[h][:, c * L + L - 1:c * L + L], EI[:, c, h:h + 1], ALU.mult, ALU.mult),
                            reads=[pb, E_t[h], EI, KT[h]], writes=[KT[h]])
                Hh = H_r.next()
                for c in range(NCH):
                    cs = slice(c * L, (c + 1) * L)
                    p1 = A.ps_rot.next()
                    mm(p1, p1[0:64, 0:64], kbf, kbf[:, cs], qk_bf[h], qk_bf[h][:, cs])
                    W = W_r.next()
                    P.op("vector", lambda e, p1=p1, W=W, c=c, cs=cs, h=h: e.scalar_tensor_tensor(
                        W[:, :], p1[0:64, 0:64], EI[:, c, h:h + 1], E_t[h][:, cs], ALU.mult, ALU.mult), reads=[p1, EI, E_t[h], W], writes=[W])
                    p2 = A.ps_rot.next()
                    mm(p2, p2[0:64, 0:DV + 1], W, W[:, :], Vp, Vp[:, c, h, :], True, False)
                    mm(p2, p2[0:64, 0:DV + 1], qt_bf[h], qt_bf[h][:, cs], C_b[h], C_b[h][:, :], False, True)
                    P.op("scalar", lambda e, p2=p2, Hh=Hh, c=c: e.copy(Hh[:, c, :], p2[0:64, 0:DV + 1]), reads=[p2, Hh], writes=[Hh])
                    p3 = A.ps_rot.next()
                    mm(p3, p3[:, 0:DV + 1], KT[h], KT[h][:, c, :], Vp, Vp[:, c, h, :])
                    P.op("vector", lambda e, p3=p3, c=c, h=h: e.scalar_tensor_tensor(
                        C_f[h][:, :], C_f[h][:, :], PL[h][:, c:c + 1], p3[:, 0:DV + 1], ALU.mult, ALU.add),
                        reads=[C_f[h], PL[h], p3], writes=[C_f[h]])
                    P.op("scalar", lambda e, h=h: e.copy(C_b[h][:, :], C_f[h][:, :]), reads=[C_f[h], C_b[h]], writes=[C_b[h]])
                den = st_r.next()
                P.op("scalar", lambda e, den=den, Hh=Hh: e.activation(den[:, :], Hh[:, :, DV], AF.Abs), reads=[Hh, den], writes=[den])
                P.op("vector", lambda e, den=den: e.tensor_scalar(den[:, :], den[:, :], 1.0, None, ALU.max), reads=[den], writes=[den])
                P.op("vector", lambda e, den=den: e.reciprocal(den[:, :], den[:, :]), reads=[den], writes=[den])
                hs = hs_r.next()
                P.op("vector", lambda e, hs=hs, Hh=Hh, den=den: e.tensor_tensor(hs[:, :, :], Hh[:, :, 0:DV], bc_last(den[:, :], DV), ALU.mult),
                     reads=[Hh, den, hs], writes=[hs])
                sq = Hh
                P.op("gpsimd", lambda e, hs=hs, sq=sq: e.tensor_tensor(sq[:, :, 0:DV], hs[:, :, :], hs[:, :, :], ALU.mult), reads=[hs, sq], writes=[sq])
                ms = st_r.next()
                P.op("vector", lambda e, ms=ms, sq=sq: e.tensor_reduce(ms[:, :], sq[:, :, 0:DV], AX.X, ALU.add), reads=[sq, ms], writes=[ms])
                rsqrt_op(P, ms, ms[:, :], ms, ms[:, :], 1.0 / DV, A.eps)
                P.op("vector", lambda e, hs=hs, ms=ms: e.tensor_tensor(hs[:, :, :], hs[:, :, :], bc_last(ms[:, :], DV), ALU.mult), reads=[hs, ms], writes=[hs])
                P.op("vector", lambda e, hs=hs, h=h: e.tensor_tensor(hs[:, :, :], hs[:, :, :], bc_mid(d["mnw_t"][:, h * DV:(h + 1) * DV], NCH), ALU.mult),
                     reads=[hs, d["mnw_t"]], writes=[hs])
                P.op("vector", lambda e, hs=hs, h=h: e.tensor_tensor(hs[:, :, :], hs[:, :, :], og[:, :, h * DV:(h + 1) * DV], ALU.mult),
                     reads=[hs, og], writes=[hs])
                finals.append(P.dma("sync", y_out[seg * SEG:(seg + 1) * SEG, 512 + h * DV:512 + (h + 1) * DV].rearrange("(c t) i -> t c i", c=NCH),
                                    hs[:, :, :], owner=hs, reads=[hs]))
    return run_seg


def _pc(v, width=128):
    return np.ascontiguousarray(np.asarray(v, np.float32).reshape(-1, width).T)


def _rep(v, n=64):
    return np.ascontiguousarray(np.broadcast_to(np.asarray(v, np.float32)[None, :], (n, len(v))))


def prep_a0(inp, half, xT_b):
    W = inp["ev_w_in"][0]
    mu = inp["rw_mu"][0]
    c0 = 512 * half
    cols = []
    mu_rkv = np.zeros((64, 24), np.float32)
    for h in range(8):
        gh = 8 * half + h
        for q in range(3):
            cols.append(np.arange(q * 1024 + gh * 64, q * 1024 + gh * 64 + 64))
            mu_rkv[:, h * 3 + q] = mu[q * 1024 + gh * 64:q * 1024 + gh * 64 + 64]
    cols = np.concatenate(cols)
    mu_l = np.zeros((128, 4), np.float32)
    mu_l[:96, 0] = mu[3072:3168]; mu_l[:96, 1] = mu[3168:3264]; mu_l[:, 2] = mu[3264:3392]; mu_l[:, 3] = mu[3392:3520]
    rwp = np.zeros((64, 5, 8), np.float32)
    for i, nm in enumerate(("rw_w0", "rw_a0", "rw_k_k", "rw_k_a")):
        rwp[:, i, :] = inp[nm][0][c0:c0 + 512].reshape(8, 64).T
    rwp[:, 4, :] = inp["rw_r_k"][0][8 * half:8 * half + 8].T
    M0 = 3520
    mh = 2 * half
    qc = [M0 + (mh + i) * 128 for i in range(2)]
    kc_ = [M0 + 512 + (mh + i) * 128 for i in range(2)]
    qk_cols = np.concatenate([np.arange(c, c + 128) for c in qc + kc_])
    cwm = inp["ml_conv_w"][0]; cbm = inp["ml_conv_b"][0]
    cidx = [(mh + 0) * 128, (mh + 1) * 128, 512 + (mh + 0) * 128, 512 + (mh + 1) * 128]
    cw = np.stack([cwm[:, c:c + 128].T for c in cidx], axis=1)
    cb = np.stack([cbm[c:c + 128] for c in cidx], axis=1)
    ifb = np.array([inp["ml_i_b"][0][mh], inp["ml_i_b"][0][mh + 1], inp["ml_f_b"][0][mh], inp["ml_f_b"][0][mh + 1]], np.float32)
    if_cols = [M0 + 3072 + mh, M0 + 3072 + mh + 1, M0 + 3076 + mh, M0 + 3076 + mh + 1]
    ca = np.ascontiguousarray
    return {
        "xT": xT_b, "norm1": _pc(inp["ev_norm1_w"][0]),
        "w_rkv": ca(W[:, cols]), "w_lora": ca(W[:, 3072:3520]), "mu_rkv": mu_rkv, "mu_l": mu_l, "rwp": rwp,
        "w2": ca(inp["rw_w2"][0][:, c0:c0 + 512]), "a2": ca(inp["rw_a2"][0][:, c0:c0 + 512]), "g2": ca(inp["rw_g2"][0][:, c0:c0 + 512]),
        "lnw": _rep(inp["rw_ln_w"][0][c0:c0 + 512]), "lnb": _rep(inp["rw_ln_b"][0][c0:c0 + 512]),
        "w_qk": ca(W[:, qk_cols]), "w_v": ca(W[:, M0 + 1024 + mh * 256:M0 + 1024 + mh * 256 + 512]),
        "w_o": ca(W[:, M0 + 2048 + mh * 256:M0 + 2048 + mh * 256 + 512]), "w_if": ca(W[:, if_cols]),
        "cw": ca(cw.astype(np.float32)), "cb": ca(cb.astype(np.float32)),
        "ifb_c": ca(ifb.reshape(4, 1)), "ifb_t": _rep(ifb), "mnw": _rep(inp["ml_norm_w"][0][mh * 256:mh * 256 + 512]),
    }


def build_phase_a1():
    nc = bass.Bass("TRN2", target_bir_lowering=False)
    dr = lambda name, shape, dt=F32: nc.dram_tensor(name, list(shape), dt, kind="ExternalInput").ap()
    xT = dr("xT", [D, T]); norm1 = dr("norm1", [128, KC])
    w_hq = dr("w_hq", [D, 512]); w_hf = dr("w_hf", [D, 512]); w_hi = dr("w_hi", [D, 512]); w_hg = dr("w_hg", [D, 512])
    lbl = dr("lbl", [128, 2, 4]); hnw = dr("hnw", [64, 512])
    w_z = dr("w_z", [D, 512]); w_xbc = dr("w_xbc", [D, 1024]); w_dt = dr("w_dt", [D, 8])
    cw = dr("cw", [128, 8, 4]); cb = dr("cb", [128, 8])
    dtp_c = dr("dtp_c", [8, 2]); dtb_t = dr("dtb_t", [64, 8]); dsk_t = dr("dsk_t", [64, 8]); snw = dr("snw", [64, 512])
    y_out = nc.dram_tensor("y_tok", [T, 1024], F32, kind="ExternalOutput").ap()

    with ExitStack() as stack:
        A = PhaseA(nc, stack)
        P = A.P
        finals = []
        lbl_t = A.small(lbl, [128, 2, 4], "lbl"); hnw_t = A.small(hnw, [64, 512], "hnw")
        cw_t = A.small(cw, [128, 8, 4], "cw"); cb_t = A.small(cb, [128, 8], "cb")
        dtp_t = A.small(dtp_c, [8, 2], "dtp_c"); dtb_t_t = A.small(dtb_t, [64, 8], "dtb_t")
        dsk_t_t = A.small(dsk_t, [64, 8], "dsk_t"); snw_t = A.small(snw, [64, 512], "snw")

        def T2(shape, dt, name, n=2):
            return Rot([P.sbt(shape, dt, f"{name}{i}") for i in range(n)])

        def act(out_t, out_ap, in_t, in_ap, func, **kw):
            rd = [in_t] + [kw.pop("bias_t")] if "bias_t" in kw else [in_t]
            P.op("scalar", lambda e: e.activation(out_ap, in_ap, func, **kw), reads=rd + [out_t], writes=[out_t])

        def tt(eng, out_t, out_ap, a_t, a_ap, b_t, b_ap, op):
            P.op(eng, lambda e: e.tensor_tensor(out_ap, a_ap, b_ap, op), reads=[a_t, b_t, out_t], writes=[out_t])

        def ts(eng, out_t, out_ap, a_t, a_ap, s1, s2, op0, op1=None, extra=()):
            if op1 is None:
                P.op(eng, lambda e: e.tensor_scalar(out_ap, a_ap, s1, None, op0), reads=[a_t, out_t] + list(extra), writes=[out_t])
            else:
                P.op(eng, lambda e: e.tensor_scalar(out_ap, a_ap, s1, s2, op0, op1), reads=[a_t, out_t] + list(extra), writes=[out_t])

        def stt(eng, out_t, out_ap, a_t, a_ap, sc, b_t, b_ap, op0, op1, extra=()):
            P.op(eng, lambda e: e.scalar_tensor_tensor(out_ap, a_ap, sc, b_ap, op0, op1),
                 reads=[a_t, b_t, out_t] + list(extra), writes=[out_t])

        def mm(pt, out_ap, l_t, l_ap, r_t, r_ap, start=True, stop=True):
            P.op("tensor", lambda e: e.matmul(out_ap, l_ap, r_ap, start=start, stop=stop), reads=[l_t, r_t], writes=[pt])

        def cp(eng, out_t, out_ap, in_t, in_ap):
            if eng == "scalar":
                P.op("scalar", lambda e: e.copy(out_ap, in_ap), reads=[in_t, out_t], writes=[out_t])
            else:
                P.op(eng, lambda e: e.tensor_copy(out_ap, in_ap), reads=[in_t, out_t], writes=[out_t])

        lb = P.sbt([128, 4], F32, "lb"); oml = P.sbt([128, 4], F32, "oml"); noml = P.sbt([128, 4], F32, "noml")
        tt("vector", lb, lb[:, :], lbl_t, lbl_t[:, 1, :], lbl_t, lbl_t[:, 0, :], ALU.subtract)
        act(lb, lb[:, :], lb, lb[:, :], AF.Sigmoid)
        ts("vector", oml, oml[:, :], lb, lb[:, :], -1.0, 1.0, ALU.mult, ALU.add)
        ts("vector", noml, noml[:, :], oml, oml[:, :], -1.0, None, ALU.mult)
        HS_f = [P.sbt([128, 128], F32, f"HS{h}") for h in range(4)]
        HS_b = [P.sbt([128, 128], BF16, f"HSb{h}") for h in range(4)]
        for h in range(4):
            P.op("gpsimd", lambda e, h=h: e.memset(HS_f[h][:, :], 0.0), writes=[HS_f[h]])
            P.op("gpsimd", lambda e, h=h: e.memset(HS_b[h][:, :], 0.0), writes=[HS_b[h]])
        hf = {k: P.sbt([128, SEG], F32, "h_" + k) for k in ("q", "sg", "lf", "kf", "b", "eb", "enb", "dnl")}
        qt_h = P.sbt([128, SEG], BF16, "h_qt"); kt_h = P.sbt([128, SEG], BF16, "h_kt"); kh_h = P.sbt([128, SEG], BF16, "h_kh")
        HKT = P.sbt([64, NCH, 128], BF16, "h_KT"); HV = P.sbt([64, NCH, 128], BF16, "h_V"); HG = P.sbt([64, NCH, 128], BF16, "h_G")
        HO = P.sbt([64, NCH, 128], F32, "h_O"); HO2 = P.sbt([64, NCH, 128], F32, "h_O2")
        W_r = T2([64, L], BF16, "h_W", 3)
        st_r = T2([64, NCH], F32, "h_st", 4)

        def hgrn_seg(seg):
            for h in range(4):
                hc = slice(h * 128, (h + 1) * 128)
                wq = A.load_w(w_hq[:, hc], 128)
                pt = A.ps_rot.next(); A.proj_cm(pt, wq, 0, 128, seg)
                q = hf["q"]; act(q, q[:, :], pt, pt[:, 0:SEG], AF.Silu)
                wf = A.load_w(w_hf[:, hc], 128)
                pt = A.ps_rot.next(); A.proj_cm(pt, wf, 0, 128, seg)
                sg = hf["sg"]; act(sg, sg[:, :], pt, pt[:, 0:SEG], AF.Sigmoid)
                lf = hf["lf"]; kf = hf["kf"]
                ts("vector", lf, lf[:, :], sg, sg[:, :], oml[:, h:h + 1], lb[:, h:h + 1], ALU.mult, ALU.add, extra=[oml, lb])
                act(lf, lf[:, :], lf, lf[:, :], AF.Ln)
                ts("vector", kf, kf[:, :], sg, sg[:, :], noml[:, h:h + 1], oml[:, h:h + 1], ALU.mult, ALU.add, extra=[oml, noml])
                b = hf["b"]
                P.op("vector", lambda e, b=b, lf=lf: e.tensor_tensor_scan(b[:, :], A.cmask2[:, :], lf[:, :], 0.0, ALU.mult, ALU.add),
                     reads=[A.cmask, lf, b], writes=[b])
                eb = hf["eb"]; enb = hf["enb"]; dnl = hf["dnl"]
                act(eb, eb[:, :], b, b[:, :], AF.Exp)
                act(enb, enb[:, :], b, b[:, :], AF.Exp, scale=-1.0)
                tt("gpsimd", dnl, v3(dnl[:, :]), b, v3(b[:, :]), b, bc_last(b[:, L - 1:SEG:L], L), ALU.subtract)
                act(dnl, dnl[:, :], dnl, dnl[:, :], AF.Exp, scale=-1.0)
                tt("vector", qt_h, qt_h[:, :], q, q[:, :], eb, eb[:, :], ALU.mult)
                tt("vector", kt_h, kt_h[:, :], kf, kf[:, :], enb, enb[:, :], ALU.mult)
                tt("gpsimd", kh_h, kh_h[:, :], kf, kf[:, :], dnl, dnl[:, :], ALU.mult)
                for cg in range(2):
                    pb = A.psb_rot.next()
                    for cc in range(4):
                        c = cg * 4 + cc
                        P.op("tensor", lambda e, pb=pb, c=c, cc=cc: e.transpose(
                            pb[0:64, cc * 128:(cc + 1) * 128], kh_h[:, c * L:(c + 1) * L], A.ident[:, :]), reads=[kh_h, A.ident], writes=[pb])
                    cp("scalar", HKT, HKT[:, cg * 4:(cg + 1) * 4, :], pb, pb[0:64, 0:512].rearrange("p (c j) -> p c j", c=4))
                wi = A.load_w(w_hi[:, hc], 128)
                for cg in range(2):
                    pt = A.ps_rot.next()
                    for cc in range(4):
                        c = cg * 4 + cc
                        A.proj_tm(pt[0:64, cc * 128:(cc + 1) * 128], pt, wi, 0, 128, seg, c * L, L)
                    cp("scalar", HV, HV[:, cg * 4:(cg + 1) * 4, :], pt, pt[0:64, 0:512].rearrange("p (c j) -> p c j", c=4))
                wg_ = A.load_w(w_hg[:, hc], 128)
                for cg in range(2):
                    pt = A.ps_rot.next()
                    for cc in range(4):
                        c = cg * 4 + cc
                        A.proj_tm(pt[0:64, cc * 128:(cc + 1) * 128], pt, wg_, 0, 128, seg, c * L, L)
                    act(HG, HG[:, cg * 4:(cg + 1) * 4, :], pt, pt[0:64, 0:512].rearrange("p (c j) -> p c j", c=4), AF.Silu)
                for c in range(NCH):
                    cs = slice(c * L, (c + 1) * L)
                    p1 = A.ps_rot.next()
                    mm(p1, p1[0:64, 0:64], kt_h, kt_h[:, cs], qt_h, qt_h[:, cs])
                    W = W_r.next()
                    tt("vector", W, W[:, :], p1, p1[0:64, 0:64], A.m_incl, A.m_incl[:, :], ALU.mult)
                    p2 = A.ps_rot.next()
                    mm(p2, p2[0:64, 0:128], W, W[:, :], HV, HV[:, c, :], True, False)
                    mm(p2, p2[0:64, 0:128], qt_h, qt_h[:, cs], HS_b[h], HS_b[h][:, :], False, True)
                    cp("scalar", HO, HO[:, c, :], p2, p2[0:64, 0:128])
                    p3 = A.ps_rot.next()
                    mm(p3, p3[:, 0:128], HKT, HKT[:, c, :], HV, HV[:, c, :])
                    stt("vector", HS_f[h], HS_f[h][:, :], HS_f[h], HS_f[h][:, :], eb[:, c * L + L - 1:c * L + L], p3, p3[:, 0:128], ALU.mult, ALU.add, extra=[eb])
                    cp("scalar", HS_b[h], HS_b[h][:, :], HS_f[h], HS_f[h][:, :])
                tt("gpsimd", HO2, HO2[:, :, :], HO, HO[:, :, :], HO, HO[:, :, :], ALU.mult)
                ms = st_r.next()
                P.op("vector", lambda e, ms=ms: e.tensor_reduce(ms[:, :], HO2[:, :, :], AX.X, ALU.add), reads=[HO2, ms], writes=[ms])
                rsqrt_op(P, ms, ms[:, :], ms, ms[:, :], 1.0 / 128, A.eps)
                tt("vector", HO2, HO2[:, :, :], HO, HO[:, :, :], ms, bc_last(ms[:, :], 128), ALU.mult)
                tt("vector", HO2, HO2[:, :, :], HO2, HO2[:, :, :], hnw_t, bc_mid(hnw_t[:, hc], NCH), ALU.mult)
                tt("vector", HO2, HO2[:, :, :], HO2, HO2[:, :, :], HG, HG[:, :, :], ALU.mult)
                finals.append(P.dma("sync", y_out[seg * SEG:(seg + 1) * SEG, hc].rearrange("(c t) i -> t c i", c=NCH),
                                    HO2[:, :, :], owner=HO2, reads=[HO2]))

        ID8 = P.sbt([64, SEG], F32, "ID8")
        P.op("vector", lambda e: e.tensor_copy(v3(ID8[:, :]), bc_mid(A.identf[:, :], NCH)), reads=[A.identf], writes=[ID8])
        Sel = P.sbt([8, 8, 128], F32, "Sel")
        for h in range(8):
            P.op("vector", lambda e, h=h: e.tensor_copy(Sel[0:8, h, :], A.identf[0:8, h:h + 1].to_broadcast([8, 128])),
                 reads=[A.identf, Sel], writes=[Sel])
        Aneg = P.sbt([8, 1], F32, "Aneg")
        act(Aneg, Aneg[:, :], dtp_t, dtp_t[:, 1:2], AF.Exp)
        ts("vector", Aneg, Aneg[:, :], Aneg, Aneg[:, :], -1.0, None, ALU.mult)
        halo = P.sbt([128, 8, 3], F32, "shalo")
        P.op("vector", lambda e: e.memset(halo[:, :, :], 0.0), writes=[halo])
        SS_f = P.sbt([128, 8, 64], F32, "SSf"); SS_b = P.sbt([128, 8, 64], BF16, "SSb"); SS_tmp = P.sbt([128, 8, 64], F32, "SStmp")
        P.op("gpsimd", lambda e: e.memset(SS_f[:, :, :], 0.0), writes=[SS_f])
        P.op("gpsimd", lambda e: e.memset(SS_b[:, :, :], 0.0), writes=[SS_b])
        raw_r = T2([128, 3 + SEG], F32, "sraw"); acc_r = Rot([hf["b"], hf["eb"]])
        xbc = [P.sbt([128, SEG], BF16, f"sxbc{i}") for i in range(8)]
        Xtok = P.sbt([64, NCH, 8, 64], BF16, "sXtok"); Xdt = P.sbt([64, NCH, 8, 64], BF16, "sXdt"); Xd2_r = T2([64, 8, 64], BF16, "sXd2", 2)
        BT = P.sbt([64, NCH, 2, 128], BF16, "sBT")
        E_h = [P.sbt([64, SEG], F32, f"sE{h}") for h in range(8)]
        Ct = [P.sbt([128, SEG], BF16, f"sCt{h}") for h in range(8)]
        dch, dz, d1, pcum = hf["q"], hf["sg"], hf["lf"], hf["kf"]
        DT = P.sbt([64, NCH, 8], F32, "sDT"); Elast = P.sbt([64, 8, NCH], F32, "sElast"); PLs = P.sbt([128, 8, NCH], F32, "sPL")
        Yt = P.sbt([64, NCH, 512], F32, "sY"); Zg = P.sbt([64, NCH, 512], BF16, "sZ"); Y2 = P.sbt([64, NCH // 2, 512], F32, "sY2")
        Ws_r = T2([64, L], BF16, "sW", 4)
        sst_r = T2([64, NCH * 2], F32, "sst", 2)

        def ssd_seg(seg):
            for blk in range(8):
                if blk % 2 == 0:
                    wx = A.load_w(w_xbc[:, blk * 128:blk * 128 + 256], 256)
                pt = A.ps_rot.next()
                A.proj_cm(pt, wx, (blk % 2) * 128, 128, seg)
                raw = raw_r.next()
                cp("vector", raw, raw[:, 0:3], halo, halo[:, blk, :])
                cp("scalar", raw, raw[:, 3:3 + SEG], pt, pt[:, 0:SEG])
                cp("vector", halo, halo[:, blk, :], raw, raw[:, SEG:SEG + 3])
                acc = acc_r.next()
                ts("vector", acc, acc[:, :], raw, raw[:, 3:3 + SEG], cw_t[:, blk, 3:4], cb_t[:, blk:blk + 1], ALU.mult, ALU.add, extra=[cw_t, cb_t])
                for tap in range(3):
                    stt("vector", acc, acc[:, :], raw, raw[:, tap:tap + SEG], cw_t[:, blk, tap:tap + 1], acc, acc[:, :], ALU.mult, ALU.add, extra=[cw_t])
                act(xbc[blk], xbc[blk][:, :], acc, acc[:, :], AF.Silu)
            if DBG_STAGE == 1:
                return
            for blk in range(6):
                for cg in range(2):
                    pb = A.psb_rot.next()
                    for cc in range(4):
                        c = cg * 4 + cc
                        P.op("tensor", lambda e, pb=pb, c=c, cc=cc, blk=blk: e.transpose(
                            pb[0:64, cc * 128:(cc + 1) * 128], xbc[blk][:, c * L:(c + 1) * L], A.ident[:, :]), reads=[xbc[blk], A.ident], writes=[pb])
                    src = pb[0:64, 0:512].rearrange("p (c j) -> p c j", c=4)
                    if blk < 4:
                        cp("scalar", Xtok, Xtok[:, cg * 4:(cg + 1) * 4, 2 * blk:2 * blk + 2, :].rearrange("p c h j -> p c (h j)"), pb, src)
                    else:
                        cp("scalar", BT, BT[:, cg * 4:(cg + 1) * 4, blk - 4, :], pb, src)
            if DBG_STAGE == 2:
                return
            wdt = A.load_w(w_dt, 8)
            pt = A.ps_rot.next()
            A.proj_cm(pt, wdt, 0, 8, seg)
            act(dch, dch[0:8, :], pt, pt[0:8, 0:SEG], AF.Exp, bias=dtp_t[:, 0:1], bias_t=dtp_t)
            act(dch, dch[0:8, :], dch, dch[0:8, :], AF.Ln, bias=A.onesf[0:8, 0:1], bias_t=A.onesf)
            act(dch, dch[0:8, :], dch, dch[0:8, :], AF.Exp, scale=Aneg[:, 0:1], bias_t=Aneg)
            tt("vector", dz, dz[0:8, :], dch, dch[0:8, :], A.cmask, A.cmask2[0:8, :], ALU.mult)
            tt("vector", d1, d1[0:8, :], dch, dch[0:8, :], dz, dz[0:8, :], ALU.subtract)
            P.op("vector", lambda e: e.tensor_tensor_scan(pcum[0:8, :], dz[0:8, :], d1[0:8, :], 0.0, ALU.mult, ALU.add), reads=[dz, d1, pcum], writes=[pcum])
            for c in range(NCH):
                pt = A.ps_rot.next()
                A.proj_tm(pt[0:64, 0:8], pt, wdt, 0, 8, seg, c * L, L)
                tt("vector", DT, DT[:, c, :], pt, pt[0:64, 0:8], dtb_t_t, dtb_t_t[:, :], ALU.add)
            act(DT, DT[:, :, :], DT, DT[:, :, :], AF.Exp)
            act(DT, DT[:, :, :], DT, DT[:, :, :], AF.Ln, bias=A.onesf[0:64, 0:1], bias_t=A.onesf)
            if DBG_STAGE == 3:
                return
            for h in range(8):
                pt = A.ps_rot.next()
                mm(pt, pt[0:64, 0:SEG], Sel, Sel[0:8, h, 0:64], dz, dz[0:8, :])
                P.op("vector", lambda e, pt=pt, h=h: e.tensor_tensor_scan(E_h[h][:, :], pt[0:64, 0:SEG], ID8[:, :], 0.0, ALU.mult, ALU.add),
                     reads=[pt, ID8, E_h[h]], writes=[E_h[h]])
                cp("vector", Elast, Elast[:, h, :], E_h[h], E_h[h][:, L - 1:SEG:L])
                if DBG_STAGE == 31:
                    continue
                pt = A.ps_rot.next()
                mm(pt, pt[:, 0:SEG], Sel, Sel[0:8, h, :], pcum, pcum[0:8, :])
                g = h // 4
                if DBG_STAGE != 32:
                    tt("vector", Ct[h], Ct[h][:, :], xbc[6 + g], xbc[6 + g][:, :], pt, pt[:, 0:SEG], ALU.mult)
                if DBG_STAGE != 33:
                    cp("vector", PLs, PLs[:, h, :], pt, pt[:, L - 1:SEG:L])
            if DBG_STAGE in (4, 31, 32, 33):
                return
            for c in range(NCH):
                tt("vector", Xdt, Xdt[:, c, :, :], Xtok, Xtok[:, c, :, :], DT, bc_last(DT[:, c, :], 64), ALU.mult)
            if DBG_STAGE == 5:
                return
            for hh in range(2):
                wz = A.load_w(w_z[:, hh * 256:(hh + 1) * 256], 256)
                for c in range(NCH):
                    pt = A.ps_rot.next()
                    A.proj_tm(pt[0:64, 0:256], pt, wz, 0, 256, seg, c * L, L)
                    act(Zg, Zg[:, c, hh * 256:(hh + 1) * 256], pt, pt[0:64, 0:256], AF.Silu)
            if DBG_STAGE == 6:
                return
            for c in range(NCH):
                cs = slice(c * L, (c + 1) * L)
                p2 = A.ps_rot.next()
                for g in range(2):
                    p1 = A.ps_rot.next()
                    mm(p1, p1[0:64, 0:64], xbc[4 + g], xbc[4 + g][:, cs], xbc[6 + g], xbc[6 + g][:, cs])
                    for hh in range(4):
                        h = g * 4 + hh
                        W = Ws_r.next()
                        tt("vector", W, W[:, :], p1, p1[0:64, 0:64], E_h[h], E_h[h][:, cs], ALU.mult)
                        mm(p2, p2[0:64, h * 64:(h + 1) * 64], W, W[:, :], Xdt, Xdt[:, c, h, :], True, False)
                        mm(p2, p2[0:64, h * 64:(h + 1) * 64], Ct[h], Ct[h][:, cs], SS_b, SS_b[:, h, :], False, True)
                cp("scalar", Yt, Yt[:, c, :], p2, p2[0:64, 0:512])
                Xd2 = Xd2_r.next()
                tt("gpsimd", Xd2, Xd2[:, :, :], Xdt, Xdt[:, c, :, :], Elast, bc_last(Elast[:, :, c], 64), ALU.mult)
                p3 = A.ps_rot.next()
                for g in range(2):
                    mm(p3, p3[:, g * 256:(g + 1) * 256], BT, BT[:, c, g, :], Xd2, Xd2[:, g * 4:(g + 1) * 4, :].rearrange("p h j -> p (h j)"))
                tt("vector", SS_tmp, SS_tmp[:, :, :], SS_f, SS_f[:, :, :], PLs, bc_last(PLs[:, :, c], 64), ALU.mult)
                tt("vector", SS_f, SS_f[:, :, :], SS_tmp, SS_tmp[:, :, :], p3, p3[:, 0:512].rearrange("p (h j) -> p h j", h=8), ALU.add)
                cp("scalar", SS_b, SS_b[:, :, :], SS_f, SS_f[:, :, :])
            if DBG_STAGE == 7:
                return
            HC = NCH // 2
            for hf_ in range(2):
                cr = slice(hf_ * HC, (hf_ + 1) * HC)
                for cc in range(HC):
                    c = hf_ * HC + cc
                    tt("gpsimd", Y2, Y2[:, cc, :].rearrange("p (h j) -> p h j", h=8), Xtok, Xtok[:, c, :, :], dsk_t_t, bc_last(dsk_t_t[:, :], 64), ALU.mult)
                tt("vector", Yt, Yt[:, cr, :], Yt, Yt[:, cr, :], Y2, Y2[:, :, :], ALU.add)
                tt("vector", Yt, Yt[:, cr, :], Yt, Yt[:, cr, :], Zg, Zg[:, cr, :], ALU.mult)
                tt("gpsimd", Y2, Y2[:, :, :], Yt, Yt[:, cr, :], Yt, Yt[:, cr, :], ALU.mult)
                ms = sst_r.next()
                P.op("vector", lambda e, ms=ms: e.tensor_reduce(ms[:, 0:HC * 2], Y2[:, :, :].rearrange("p c (g j) -> p (c g) j", g=2), AX.X, ALU.add),
                     reads=[Y2, ms], writes=[ms])
                rsqrt_op(P, ms, ms[:, 0:HC * 2], ms, ms[:, 0:HC * 2], 1.0 / 256, A.eps)
                tt("vector", Y2, Y2[:, :, :].rearrange("p c (g j) -> p (c g) j", g=2), Yt, Yt[:, cr, :].rearrange("p c (g j) -> p (c g) j", g=2),
                   ms, bc_last(ms[:, 0:HC * 2], 256), ALU.mult)
                tt("vector", Y2, Y2[:, :, :], Y2, Y2[:, :, :], snw_t, bc_mid(snw_t[:, :], HC), ALU.mult)
                r0 = seg * SEG + hf_ * HC * L
                finals.append(P.dma("sync", y_out[r0:r0 + HC * L, 512:1024].rearrange("(c t) i -> t c i", c=HC),
                                    Y2[:, :, :], owner=Y2, reads=[Y2]))

        for seg in range(NSEG):
            A.compute_xn(xT, norm1, seg)
            if DBG_ONLY in ("", "ssd"):
                ssd_seg(seg)
            if DBG_ONLY in ("", "hgrn"):
                hgrn_seg(seg)
        P.emit(final_waits=finals)
    return nc


def prep_a1(inp, half, xT_b):
    W = inp["od_w_in"][0]
    c0 = 512 * half
    ca = np.ascontiguousarray
    lbl = inp["hg_lb_logits"][:, c0:c0 + 512].reshape(2, 4, 128).transpose(2, 0, 1)
    S0 = 4096
    g0 = 2 * half
    xcols = np.arange(S0 + 1024 + c0, S0 + 1024 + c0 + 512)
    bcols = np.arange(S0 + 2048 + g0 * 128, S0 + 2048 + g0 * 128 + 256)
    ccols = np.arange(S0 + 2560 + g0 * 128, S0 + 2560 + g0 * 128 + 256)
    allc = np.concatenate([xcols, bcols, ccols])
    conv_idx = allc - (S0 + 1024)
    cwm = inp["mb_conv_w"][0]; cbm = inp["mb_conv_b"][0]
    cw = cwm[:, conv_idx].reshape(4, 8, 128).transpose(2, 1, 0)
    cb = cbm[conv_idx].reshape(8, 128).T
    hs = slice(8 * half, 8 * half + 8)
    dtp = np.stack([inp["mb_dt_bias"][0][hs], inp["mb_A_log"][0][hs]], axis=1)
    return {
        "xT": xT_b, "norm1": _pc(inp["od_norm1_w"][0]),
        "w_hq": ca(W[:, c0:c0 + 512]), "w_hf": ca(W[:, 1024 + c0:1024 + c0 + 512]),
        "w_hi": ca(W[:, 2048 + c0:2048 + c0 + 512]), "w_hg": ca(W[:, 3072 + c0:3072 + c0 + 512]),
        "lbl": ca(lbl.astype(np.float32)), "hnw": _rep(inp["hg_norm_w"][0][c0:c0 + 512]),
        "w_z": ca(W[:, S0 + c0:S0 + c0 + 512]), "w_xbc": ca(W[:, allc]), "w_dt": ca(W[:, S0 + 3072 + 8 * half:S0 + 3072 + 8 * half + 8]),
        "cw": ca(cw.astype(np.float32)), "cb": ca(cb.astype(np.float32)),
        "dtp_c": ca(dtp.astype(np.float32)), "dtb_t": _rep(inp["mb_dt_bias"][0][hs]), "dsk_t": _rep(inp["mb_D"][0][hs]),
        "snw": _rep(inp["mb_norm_w"][0][c0:c0 + 512]),
    }


_PROGS = {}


def _prog(key, fn):
    if key not in _PROGS:
        _PROGS[key] = fn()
    return _PROGS[key]


def _run(nc, maps):
    return run_bass_kernel_spmd(nc, maps, core_ids=list(range(8))).results


def _assemble_yT(ys, th):
    y = np.empty((T, 2048), np.float32)
    for half in range(2):
        y[:, 512 * half:512 * half + 512] = ys[half][:, 0:512]
        y[:, 1024 + 512 * half:1024 + 512 * half + 512] = ys[half][:, 512:1024]
    return np.ascontiguousarray(y[th * 1024:(th + 1) * 1024].T)


def kernel(**inp):
    inp = {k: np.asarray(v) for k, v in inp.items()}
    ca = np.ascontiguousarray
    x = inp["x"]
    xT = [ca(x[b].T) for b in range(NB)]
    nc = _prog("a0", build_phase_a0)
    res = _run(nc, [prep_a0(inp, c % 2, xT[c // 2]) for c in range(8)])
    ys = [r["y_tok"] for r in res]
    nc = _prog("b0", lambda: build_phase_b(1024, 1, 5632, False, False))
    maps = []
    for c in range(8):
        b, th = c // 2, c % 2
        maps.append({"xT": ca(xT[b][:, th * 1024:(th + 1) * 1024]), "yT": _assemble_yT(ys[2 * b:2 * b + 2], th),
                     "w_out": inp["ev_w_out"][0], "norm2": _pc(inp["ev_norm2_w"][0]),
                     "wg": inp["ffn_w_gate"], "wu": inp["ffn_w_up"], "wd": inp["ffn_w_down"]})
    res = _run(nc, maps)
    x1T = [ca(np.concatenate([res[2 * b]["outT"], res[2 * b + 1]["outT"]], axis=1)) for b in range(NB)]
    nc = _prog("a1", build_phase_a1)
    res = _run(nc, [prep_a1(inp, c % 2, x1T[c // 2]) for c in range(8)])
    ys = [r["y_tok"] for r in res]
    nc = _prog("b1", lambda: build_phase_b(1024, 8, 2816, True, True))
    maps = []
    for c in range(8):
        b, th = c // 2, c % 2
        maps.append({"xT": ca(x1T[b][:, th * 1024:(th + 1) * 1024]), "yT": _assemble_yT(ys[2 * b:2 * b + 2], th),
                     "w_out": inp["od_w_out"][0], "norm2": _pc(inp["od_norm2_w"][0]),
                     "wg": inp["moe_w_gate"][0], "wu": inp["moe_w_up"][0], "wd": inp["moe_w_down"][0],
                     "router": inp["moe_router"][0], "fnorm": _pc(inp["final_norm_w"])})
    res = _run(nc, maps)
    out = np.empty((NB, T, D), np.float32)
    for c in range(8):
        b, th = c // 2, c % 2
        out[b, th * 1024:(th + 1) * 1024, :] = res[c]["outT"].T
    return out
```
